# Optimizing a Trainium2 kernel written in Bass

```python
import math
import jax, jax.numpy as jnp
from jax import lax
import numpy as np

D_MODEL = 2048
BATCH = 1
SEQ = 8192
DEPTH = 1

D_MIX = D_MODEL
CONV_CH = D_MIX // 2
HEAD_DIM = 64
N_Q_HEADS = (D_MIX - CONV_CH) // HEAD_DIM
N_KV_HEADS = 2
GQA_GROUP = N_Q_HEADS // N_KV_HEADS
ATTN_CH = N_Q_HEADS * HEAD_DIM
KV_CH = N_KV_HEADS * HEAD_DIM
IN_COLS = 2 * CONV_CH + ATTN_CH + 2 * KV_CH
CONV_K = 31
WINDOW = 128
BLOCK = 128
ROT_DIM = HEAD_DIM // 4
ROPE_THETA = 500000.0
EPS = 1e-6
N_KEYS = 128
N_EXPERTS = N_KEYS * N_KEYS
PEER_HEADS = 8
PEER_KEY_DIM = 256
PEER_HALF = PEER_KEY_DIM // 2
PEER_TOPK = 16
PEER_CHUNK = 128

kernel_name = "hymba_conv_swa_peer_layer"


def rms_norm(x, g):
    xf = x.astype(jnp.float32)
    y = xf * lax.rsqrt(jnp.mean(xf * xf, axis=-1, keepdims=True) + EPS)
    return (y * g.astype(jnp.float32)).astype(x.dtype)


def layer_norm(x, g, b):
    xf = x.astype(jnp.float32)
    mu = jnp.mean(xf, axis=-1, keepdims=True)
    var = jnp.mean(jnp.square(xf - mu), axis=-1, keepdims=True)
    y = (xf - mu) * lax.rsqrt(var + EPS)
    return (y * g.astype(jnp.float32) + b.astype(jnp.float32)).astype(x.dtype)


def partial_rotary(t, cos, sin):
    half = ROT_DIM // 2
    r1 = t[..., :half]
    r2 = t[..., half:ROT_DIM]
    return jnp.concatenate([r1 * cos - r2 * sin, r2 * cos + r1 * sin, t[..., ROT_DIM:]], axis=-1)


def conv_group(u, conv_dw_w, conv_dw_b, conv_ln_g, conv_ln_b):
    a, gate = jnp.split(u, 2, axis=-1)
    h = a * jax.nn.sigmoid(gate)
    h = lax.conv_general_dilated(
        h, conv_dw_w[:, None, :].astype(h.dtype), window_strides=(1,),
        padding=[(CONV_K - 1, 0)], dimension_numbers=("NWC", "WIO", "NWC"),
        feature_group_count=CONV_CH) + conv_dw_b
    h = layer_norm(h, conv_ln_g, conv_ln_b)
    return jax.nn.silu(h)


def swa_group(q, k, v, positions, q_norm_g, k_norm_g, attn_sinks):
    B, S, _ = q.shape
    nb = S // BLOCK
    q = rms_norm(q.reshape(B, S, N_Q_HEADS, HEAD_DIM), q_norm_g)
    k = rms_norm(k.reshape(B, S, N_KV_HEADS, HEAD_DIM), k_norm_g)
    v = v.reshape(B, S, N_KV_HEADS, HEAD_DIM)
    inv_freq = ROPE_THETA ** (-jnp.arange(0, ROT_DIM, 2, dtype=jnp.float32) / ROT_DIM)
    ang = positions.astype(jnp.float32)[..., None] * inv_freq
    cos = jnp.cos(ang)[:, :, None, :].astype(q.dtype)
    sin = jnp.sin(ang)[:, :, None, :].astype(q.dtype)
    q = partial_rotary(q, cos, sin)
    k = partial_rotary(k, cos, sin)

    qb = q.reshape(B, nb, BLOCK, N_KV_HEADS, GQA_GROUP, HEAD_DIM)
    pad = ((0, 0), (BLOCK, 0), (0, 0), (0, 0))
    kb = jnp.pad(k, pad).reshape(B, nb + 1, BLOCK, N_KV_HEADS, HEAD_DIM)
    vb = jnp.pad(v, pad).reshape(B, nb + 1, BLOCK, N_KV_HEADS, HEAD_DIM)
    k_band = jnp.concatenate([kb[:, :-1], kb[:, 1:]], axis=2)
    v_band = jnp.concatenate([vb[:, :-1], vb[:, 1:]], axis=2)

    scores = jnp.einsum("bnqhgd,bnshd->bnhgqs", qb, k_band).astype(jnp.float32)
    scores = scores * (1.0 / math.sqrt(HEAD_DIM))
    blk = jnp.arange(nb)[:, None, None]
    qpos = blk * BLOCK + jnp.arange(BLOCK)[None, :, None]
    kpos = (blk - 1) * BLOCK + jnp.arange(2 * BLOCK)[None, None, :]
    mask = (kpos <= qpos) & (qpos - kpos < WINDOW) & (kpos >= 0)
    scores = jnp.where(mask[None, :, None, None], scores, -1e30)

    sink = attn_sinks.astype(jnp.float32).reshape(1, 1, N_KV_HEADS, GQA_GROUP, 1, 1)
    m = jnp.maximum(jnp.max(scores, axis=-1, keepdims=True), sink)
    p = jnp.exp(scores - m)
    denom = jnp.sum(p, axis=-1, keepdims=True) + jnp.exp(sink - m)
    probs = (p / denom).astype(v.dtype)
    out = jnp.einsum("bnhgqs,bnshd->bnqhgd", probs, v_band)
    return out.reshape(B, S, ATTN_CH)


def peer(xn, peer_w_q, peer_sub_keys, peer_u, peer_v):
    B, S, D = xn.shape
    T = B * S
    xt = xn.reshape(T, D)
    qh = (xt @ peer_w_q).reshape(T, PEER_HEADS, 2, PEER_HALF)
    sub = jnp.einsum("thpc,hpnc->thpn", qh, peer_sub_keys).astype(jnp.float32)
    v1, i1 = lax.top_k(sub[:, :, 0], PEER_TOPK)
    v2, i2 = lax.top_k(sub[:, :, 1], PEER_TOPK)
    cand = (v1[..., :, None] + v2[..., None, :]).reshape(T, PEER_HEADS, PEER_TOPK * PEER_TOPK)
    vals, cidx = lax.top_k(cand, PEER_TOPK)
    e1 = jnp.take_along_axis(i1, cidx // PEER_TOPK, axis=-1)
    e2 = jnp.take_along_axis(i2, cidx % PEER_TOPK, axis=-1)
    experts = e1 * N_KEYS + e2
    gates = jax.nn.softmax(vals, axis=-1).astype(xn.dtype)

    nc = T // PEER_CHUNK

    def chunk_fn(args):
        xc, ec, gc = args
        u_sel = jnp.take(peer_u, ec, axis=0)
        h = jnp.einsum("cd,chkd->chk", xc, u_sel)
        a = jax.nn.gelu(h, approximate=False) * gc
        v_sel = jnp.take(peer_v, ec, axis=0)
        return jnp.einsum("chk,chkd->cd", a, v_sel)

    y = lax.map(chunk_fn, (xt.reshape(nc, PEER_CHUNK, D),
                           experts.reshape(nc, PEER_CHUNK, PEER_HEADS, PEER_TOPK),
                           gates.reshape(nc, PEER_CHUNK, PEER_HEADS, PEER_TOPK)))
    return y.reshape(B, S, D)


def setup_inputs(seed: int = 0) -> dict:
    key = jax.random.key(seed)
    ks = jax.random.split(key, 20)
    f32 = jnp.float32
    nrm = lambda k, shape, s: jax.random.normal(k, shape, f32) * s
    positions = jnp.broadcast_to(jnp.arange(SEQ, dtype=jnp.int32)[None, :], (BATCH, SEQ))
    return {
        "x": nrm(ks[0], (BATCH, SEQ, D_MODEL), 1.0),
        "positions": positions,
        "norm1_g": 1.0 + nrm(ks[1], (D_MODEL,), 0.02),
        "w_in": nrm(ks[2], (D_MODEL, IN_COLS), D_MODEL ** -0.5),
        "b_in": nrm(ks[3], (IN_COLS,), 0.02),
        "conv_dw_w": nrm(ks[4], (CONV_K, CONV_CH), CONV_K ** -0.5),
        "conv_dw_b": nrm(ks[5], (CONV_CH,), 0.02),
        "conv_ln_g": 1.0 + nrm(ks[6], (CONV_CH,), 0.02),
        "conv_ln_b": nrm(ks[7], (CONV_CH,), 0.02),
        "q_norm_g": 1.0 + nrm(ks[8], (HEAD_DIM,), 0.02),
        "k_norm_g": 1.0 + nrm(ks[9], (HEAD_DIM,), 0.02),
        "attn_sinks": nrm(ks[10], (N_Q_HEADS,), 0.5),
        "w_out": nrm(ks[11], (D_MIX, D_MODEL), D_MIX ** -0.5),
        "norm2_g": 1.0 + nrm(ks[12], (D_MODEL,), 0.02),
        "peer_w_q": nrm(ks[13], (D_MODEL, PEER_HEADS * PEER_KEY_DIM), D_MODEL ** -0.5),
        "peer_sub_keys": nrm(ks[14], (PEER_HEADS, 2, N_KEYS, PEER_HALF), PEER_HALF ** -0.5),
        "peer_u": nrm(ks[15], (N_EXPERTS, D_MODEL), D_MODEL ** -0.5),
        "peer_v": nrm(ks[16], (N_EXPERTS, D_MODEL), PEER_HEADS ** -0.5),
    }


def reference(x, positions, norm1_g, w_in, b_in, conv_dw_w, conv_dw_b, conv_ln_g, conv_ln_b,
              q_norm_g, k_norm_g, attn_sinks, w_out, norm2_g, peer_w_q, peer_sub_keys,
              peer_u, peer_v):
    for _ in range(DEPTH):
        xn = rms_norm(x, norm1_g)
        proj = xn @ w_in + b_in
        c0 = 2 * CONV_CH
        conv_in = proj[..., :c0]
        q = proj[..., c0:c0 + ATTN_CH]
        k = proj[..., c0 + ATTN_CH:c0 + ATTN_CH + KV_CH]
        v = proj[..., c0 + ATTN_CH + KV_CH:]
        conv_out = conv_group(conv_in, conv_dw_w, conv_dw_b, conv_ln_g, conv_ln_b)
        attn_out = swa_group(q, k, v, positions, q_norm_g, k_norm_g, attn_sinks)
        mixed = jnp.concatenate([conv_out, attn_out], axis=-1)
        x = x + mixed @ w_out
        x = x + peer(rms_norm(x, norm2_g), peer_w_q, peer_sub_keys, peer_u, peer_v)
    return x
```

```python
import contextlib
import numpy as np
import concourse.bass as bass
import concourse.mybir as mybir
from concourse.bass_utils import run_bass_kernel_spmd

F32 = mybir.dt.float32
BF16 = mybir.dt.bfloat16
U32 = mybir.dt.uint32
I32 = mybir.dt.int32
AF = mybir.ActivationFunctionType
ALU = mybir.AluOpType
AX = mybir.AxisListType

DSIZE = {F32: 4, BF16: 2, U32: 4, I32: 4, mybir.dt.uint16: 2, mybir.dt.uint8: 1}


class Sched:
    ENGS = ("pe", "act", "dve", "pool", "sp")

    def __init__(self, nc, stack, same_engine_sync=True, n_dma_sems=24):
        self.nc = nc
        self.eng_obj = {"pe": nc.tensor, "act": nc.scalar, "dve": nc.vector,
                        "pool": nc.gpsimd, "sp": nc.sync}
        self.prog = {e: [] for e in self.ENGS}
        self.sem = {e: stack.enter_context(nc.semaphore("s_" + e)) for e in self.ENGS}
        self.cnt = {e: 0 for e in self.ENGS}
        self.seen = {e: {} for e in self.ENGS}
        self.same_engine_sync = same_engine_sync
        self.dma_sems = [stack.enter_context(nc.semaphore("d%d" % i)) for i in range(2 * n_dma_sems)]
        self.dma_val = [0] * (2 * n_dma_sems)
        self.dma_rr = {"sp": 0, "pool": 0}
        self.n_dma_sems = n_dma_sems
        self.semkey = {}
        for e in self.ENGS:
            self.semkey[("e", e)] = self.sem[e]
        for i, s in enumerate(self.dma_sems):
            self.semkey[("d", i)] = s
        self.records = {"sb": [], "ps": [], "dr": []}
        self.tinfo = {}
        self.sb_off = 16640
        self.sb_stack = []
        pt = nc.alloc_psum_tensor("psum_all", [128, 8, 512], F32)
        self.psum = pt.ap()
        self.tinfo[pt.name] = ("ps", 0, 4)
        self.n_inst = 0
        self.out_tokens = []

    def sb(self, name, shape, dtype, align=64):
        nbytes = int(np.prod(shape[1:])) * DSIZE[dtype]
        off = (self.sb_off + align - 1) // align * align
        t = self.nc.alloc_sbuf_tensor_at(name, list(shape), dtype, offset=off)
        self.sb_off = off + nbytes
        assert self.sb_off <= 229344, ("SBUF overflow", name, self.sb_off)
        self.tinfo[t.name] = ("sb", off, DSIZE[dtype])
        return t.ap()

    def sb_at(self, name, shape, dtype, off):
        t = self.nc.alloc_sbuf_tensor_at(name, list(shape), dtype, offset=off)
        self.tinfo[t.name] = ("sb", off, DSIZE[dtype])
        return t.ap()

    def mark(self):
        self.sb_stack.append(self.sb_off)

    def release(self):
        self.sb_off = self.sb_stack.pop()

    def dram(self, name, shape, dtype, kind):
        t = self.nc.dram_tensor(name, list(shape), dtype, kind=kind)
        self.tinfo[t.name] = ("dr", 0, DSIZE[dtype])
        return t.ap()

    def reg_tensor(self, name, space, base, dsize):
        self.tinfo[name] = (space, base, dsize)

    def region(self, ap):
        name = ap.tensor.name
        space, base, dsz = self.tinfo[name]
        dsz = DSIZE[ap.dtype]
        dims = [tuple(d) for d in ap.ap]
        if space == "dr":
            off = ap.offset
            lo = off
            hi = off + sum((c - 1) * abs(s) for s, c in dims) + 1
            return (name, lo * dsz, hi * dsz)
        pstride = dims[0][0]
        off = ap.offset
        if pstride > 0:
            off = off % pstride
        lo = off
        hi = off + sum((c - 1) * abs(s) for s, c in dims[1:]) + 1
        lo_b = base + lo * dsz
        hi_b = base + hi * dsz
        if space == "ps":
            lo_b = lo_b // 2048 * 2048
            hi_b = (hi_b + 2047) // 2048 * 2048
        return (space, lo_b, hi_b)

    def _deps(self, reads, writes):
        deps = {}
        def add(tok):
            k, v = tok
            if deps.get(k, -1) < v:
                deps[k] = v
        rregs = [self.region(a) for a in reads]
        wregs = [self.region(a) for a in writes]
        for (sp, lo, hi) in rregs:
            for rec in self.records_for(sp):
                if rec[3] and rec[0] == sp and rec[1] < hi and lo < rec[2]:
                    add(rec[4])
        for (sp, lo, hi) in wregs:
            for rec in self.records_for(sp):
                if rec[0] == sp and rec[1] < hi and lo < rec[2]:
                    add(rec[4])
        return deps, rregs, wregs

    def records_for(self, sp):
        if sp in ("sb", "ps"):
            return self.records[sp]
        return self.records["dr"]

    def _commit(self, rregs, wregs, tok):
        for (sp, lo, hi) in wregs:
            lst = self.records_for(sp)
            lst[:] = [r for r in lst if not (r[0] == sp and lo <= r[1] and r[2] <= hi)]
            lst.append((sp, lo, hi, True, tok))
        for (sp, lo, hi) in rregs:
            lst = self.records_for(sp)
            for i, r in enumerate(lst):
                if (not r[3]) and r[0] == sp and r[1] == lo and r[2] == hi and r[4][0] == tok[0]:
                    lst[i] = (sp, lo, hi, False, tok)
                    break
            else:
                lst.append((sp, lo, hi, False, tok))

    def _emit_waits(self, eng, deps):
        for k, v in deps.items():
            if k == ("e", eng):
                if eng == "pe" or not self.same_engine_sync:
                    continue
            if self.seen[eng].get(k, 0) >= v:
                continue
            self.seen[eng][k] = v
            self.prog[eng].append(("wait", k, v))

    def op(self, eng, fn, reads=(), writes=(), extra_deps=()):
        deps, rregs, wregs = self._deps(reads, writes)
        for tok in extra_deps:
            if deps.get(tok[0], -1) < tok[1]:
                deps[tok[0]] = tok[1]
        self._emit_waits(eng, deps)
        self.cnt[eng] += 1
        tok = (("e", eng), self.cnt[eng])
        self.prog[eng].append(("inst", fn, ("e", eng), 1))
        self._commit(rregs, wregs, tok)
        self.n_inst += 1
        return tok

    def dma(self, eng, out, in_, is_output=False, **kw):
        deps, rregs, wregs = self._deps([in_], [out])
        qn = "pool" if eng == "pool" else "sp"
        i = self.dma_rr[qn] + (self.n_dma_sems if qn == "pool" else 0)
        self.dma_rr[qn] = (self.dma_rr[qn] + 1) % self.n_dma_sems
        k = ("d", i)
        if self.dma_val[i] > 0:
            if deps.get(k, -1) < self.dma_val[i]:
                deps[k] = self.dma_val[i]
        self._emit_waits(eng, deps)
        self.dma_val[i] += 16
        tok = (k, self.dma_val[i])

        def fn(e, out=out, in_=in_, kw=kw):
            return e.dma_start(out=out, in_=in_, **kw)
        self.prog[eng].append(("inst", fn, k, 16))
        self._commit(rregs, wregs, tok)
        self.n_inst += 1
        if is_output:
            self.out_tokens.append(tok)
        return tok

    def finish(self):
        deps = {}
        for k, v in self.out_tokens:
            if deps.get(k, -1) < v:
                deps[k] = v
        self._emit_waits("sp", deps)

    def replay(self, block):
        nc = self.nc
        semkey = self.semkey

        def mk(engname):
            def body(e):
                for item in self.prog[engname]:
                    if item[0] == "wait":
                        e.wait_ge(semkey[item[1]], item[2])
                    else:
                        inst = item[1](e)
                        inst.then_inc(semkey[item[2]], item[3])
            return body
        block.tensor(mk("pe"))
        block.scalar(mk("act"))
        block.vector(mk("dve"))
        block.gpsimd(mk("pool"))
        block.sync(mk("sp"))


NCORES = 8
D = 2048
SEQ = 8192
TOK = SEQ // NCORES
NT = TOK // 128
NTH = NT + 1
TH = TOK + 128
DC = D // 128
CONV_CH = 1024
CONV_K = 31
EPS = 1e-6
NKEYS = 128
PH = 8
TWO_PI = 6.283185307179586
CW1 = 6.28125
CW2 = TWO_PI - CW1


class Builder:
    def __init__(self, nc, stack, debug=None, stop_after=None):
        self.nc = nc
        self.s = Sched(nc, stack)
        self.debug = debug or []
        self.stop_after = stop_after
        self.dbg_outs = {}

    def mm(self, out, lhsT, rhs, start=True, stop=True):
        return self.s.op("pe", lambda e: e.matmul(out, lhsT=lhsT, rhs=rhs, start=start, stop=stop),
                         reads=[lhsT, rhs] + ([] if start else [out]), writes=[out])

    def tr(self, out, in_, ident):
        return self.s.op("pe", lambda e: e.transpose(out, in_, ident), reads=[in_, ident], writes=[out])

    def act(self, out, in_, func, bias=None, scale=None, accum=None, eng="act"):
        kw = {}
        reads = [in_]
        if bias is not None:
            kw["bias"] = bias
            if not isinstance(bias, (int, float)):
                reads.append(bias)
        if scale is not None:
            kw["scale"] = scale
            if not isinstance(scale, (int, float)):
                reads.append(scale)
        writes = [out]
        if accum is not None:
            kw["accum_out"] = accum
            writes.append(accum)
        return self.s.op(eng, lambda e: e.activation(out=out, in_=in_, func=func, **kw), reads=reads, writes=writes)

    def tt(self, out, in0, in1, op, eng="dve"):
        return self.s.op(eng, lambda e: e.tensor_tensor(out=out, in0=in0, in1=in1, op=op), reads=[in0, in1], writes=[out])

    def ts(self, out, in0, s1, s2, op0, op1=None, eng="dve"):
        reads = [in0]
        for x in (s1, s2):
            if x is not None and not isinstance(x, (int, float)):
                reads.append(x)
        if op1 is None:
            return self.s.op(eng, lambda e: e.tensor_scalar(out=out, in0=in0, scalar1=s1, scalar2=None, op0=op0), reads=reads, writes=[out])
        return self.s.op(eng, lambda e: e.tensor_scalar(out=out, in0=in0, scalar1=s1, scalar2=s2, op0=op0, op1=op1), reads=reads, writes=[out])

    def stt(self, out, in0, scalar, in1, op0, op1):
        reads = [in0, in1]
        if not isinstance(scalar, (int, float)):
            reads.append(scalar)
        return self.s.op("dve", lambda e: e.scalar_tensor_tensor(out=out, in0=in0, scalar=scalar, in1=in1, op0=op0, op1=op1), reads=reads, writes=[out])

    def cp(self, out, in_, eng="dve"):
        if eng == "act":
            return self.s.op("act", lambda e: e.copy(out=out, in_=in_), reads=[in_], writes=[out])
        return self.s.op(eng, lambda e: e.tensor_copy(out=out, in_=in_), reads=[in_], writes=[out])

    def memset(self, ap, val, eng="dve"):
        return self.s.op(eng, lambda e: e.memset(ap, val), writes=[ap])

    def recip(self, out, in_):
        return self.s.op("dve", lambda e: e.reciprocal(out=out, in_=in_), reads=[in_], writes=[out])

    def rsum(self, out, in_):
        return self.s.op("dve", lambda e: e.reduce_sum(out=out, in_=in_, axis=AX.X), reads=[in_], writes=[out])

    def dbg(self, name, ap_sb, shape, dtype=F32):
        if name not in self.debug:
            return
        d = self.s.dram("dbg_" + name, list(shape), dtype, "ExternalOutput")
        self.s.dma("sp", d, ap_sb, is_output=True)
        self.dbg_outs[name] = "dbg_" + name

    def norm_part(self, xt, gbc, ss_col, rstd_col, junk, xnb):
        self.act(junk, xt, AF.Square, accum=ss_col)
        self.ts(rstd_col, ss_col, 1.0 / D, EPS, ALU.mult, ALU.add)
        self.act(rstd_col, rstd_col, AF.Sqrt)
        self.recip(rstd_col, rstd_col)
        self.stt(xnb, xt, rstd_col, gbc, ALU.mult, ALU.mult)

    def transpose_part(self, xnb, dstT, col0, ident_bf, psb):
        s = self.s
        for half in range(2):
            pb = s.psum[:, psb + half, :].bitcast(BF16).rearrange("p (a b) -> p a b", a=8)
            for i in range(8):
                c = half * 8 + i
                self.tr(pb[:, i, :], xnb[:, c * 128:(c + 1) * 128], ident_bf)
            self.cp(dstT[:, half * 8:(half + 1) * 8, col0:col0 + 128], pb, eng="act" if half == 0 else "dve")


def build_program(debug=None, stop_after=None):
    nc = bass.Bass("TRN2", target_bir_lowering=False)
    with contextlib.ExitStack() as stack:
        b = Builder(nc, stack, debug=debug, stop_after=stop_after)
        _emit(b)
        b.s.finish()
        with nc.Block() as block:
            b.s.replay(block)
    return nc, b


def _emit(b):
    s = b.s
    nc = b.nc
    dr = lambda name, shape, dt=F32: s.dram(name, shape, dt, "ExternalInput")
    xin = dr("xin", [TH, D])
    pos = dr("pos", [128, NTH], I32)
    g1bc_d = dr("g1bc", [128, D])
    g2bc_d = dr("g2bc", [128, D])
    wconv_d = dr("wconv", [8, 128, DC, 256])
    wqkv_d = dr("wqkv", [128, DC, 1280])
    bconv_d = dr("bconv", [128, 16])
    bqkv_d = dr("bqkvbc", [128, 1280])
    cw_d = dr("convw", [128, 8, CONV_K])
    cvec_d = dr("convvec", [128, 3, 8])
    qkg_d = dr("qkgbc", [128, 2, 64])
    sink_d = dr("sinkbc", [128, 16])
    invf_d = dr("invfbc", [128, 8])
    masks_d = dr("masks", [128, 4, 128])
    halo_d = dr("halomask", [128, 128])
    wout_d = dr("wout", [4, 128, DC, 512])
    wq_d = dr("wq", [16, 128, DC, 128])
    keys_d = dr("keysT", [128, 16, 128])
    ut_d = dr("ut", [128, 128, DC, 128])
    v_d = dr("pv", [128 * 128, D])
    iota_d = dr("iotas", [128, 128 + 2048])
    y_d = s.dram("y", [TOK, D], F32, "ExternalOutput")
    x1_d = s.dram("x1scratch", [TOK, D], F32, "Internal")
    g_d = s.dram("gscratch", [128, 128, TOK], BF16, "Internal")

    ident_f = s.sb("ident_f", [128, 128], F32)
    ident_b = s.sb("ident_b", [128, 128], BF16)
    iota128 = s.sb("iota128", [128, 128], F32)
    onesm = s.sb("onesm", [128, 128], F32)
    small = s.sb("small", [128, 64], F32)
    s.dma("sp", iota128, iota_d[:, 0:128])
    pidx = s.sb("pidx", [128, 1], F32)
    s.op("pool", lambda e: e.iota(pidx, pattern=[[0, 1]], base=0, channel_multiplier=1,
                                  allow_small_or_imprecise_dtypes=True), writes=[pidx])
    b.ts(ident_f, iota128, pidx, None, ALU.is_equal)
    b.cp(ident_b, ident_f)
    b.memset(onesm, 1.0 / CONV_CH)

    PS = s.psum
    r0 = (s.sb_off + 63) // 64 * 64
    wq_s = s.sb("wq_s", [128, DC, 1024], BF16)
    xn2T = s.sb_at("xn2T", [128, DC, TOK], BF16, r0)
    s.mark()
    mixedT = s.sb("mixedT", [128, DC, TOK], BF16)
    s.mark()
    xnT = s.sb("xnT", [128, DC, TH], BF16)
    wkv_s = s.sb("wkv_s", [128, DC, 256], BF16)
    g1bc = s.sb("g1bc_s", [128, D], F32)
    s.dma("sp", g1bc, g1bc_d)
    ss1 = s.sb("ss1", [128, NTH], F32)
    rs1 = s.sb("rs1", [128, NTH], F32)
    s.mark()
    xts = [s.sb("xt%d" % i, [128, D], F32) for i in range(4)]
    xnbs = [s.sb("xnb%d" % i, [128, D], BF16) for i in range(2)]
    junk = s.sb("junk", [128, D], BF16)
    def part_a(j):
        xt = xts[j % 4]
        s.dma("sp", xt, xin[j * 128:(j + 1) * 128, :])
        b.norm_part(xt, g1bc, ss1[:, j:j + 1], rs1[:, j:j + 1], junk, xnbs[j % 2])

    part_a(0)
    for j in range(NTH):
        if j + 1 < NTH:
            part_a(j + 1)
        b.transpose_part(xnbs[j % 2], xnT, j * 128, ident_b, (j % 2) * 2)
    s.release()
    b.dbg("xnT", xnT, [128, DC, TH], BF16)
    if b.stop_after == "1a":
        return

    s.mark()
    h = s.sb("h", [128, 8, TH], BF16)
    bconv = s.sb("bconv_s", [128, 16], F32)
    s.dma("sp", bconv, bconv_d)
    halo = s.sb("halo_s", [128, 128], BF16)
    s.dma("pool", halo, halo_d)
    s.mark()
    wcb = [s.sb("wcb%d" % i, [128, DC, 256], BF16) for i in range(2)]
    sig = [s.sb("sig%d" % i, [128, 512], F32) for i in range(2)]
    TR = [(0, 512), (512, 1024), (1024, TH)]
    k = 0
    for cp in range(8):
        w = wcb[cp % 2]
        s.dma("pool", w, wconv_d[cp])
        if cp == 1:
            for i in range(4):
                s.dma("pool", wq_s[:, :, i * 256:(i + 1) * 256], wqkv_d[:, :, i * 256:(i + 1) * 256])
            s.dma("pool", wkv_s, wqkv_d[:, :, 1024:1280])
        for (t0, t1) in TR:
            n = t1 - t0
            pa = PS[:, (k % 2) * 2, 0:n]
            pg = PS[:, (k % 2) * 2 + 1, 0:n]
            for c in range(DC):
                b.mm(pa, w[:, c, 0:128], xnT[:, c, t0:t1], start=(c == 0), stop=(c == DC - 1))
            for c in range(DC):
                b.mm(pg, w[:, c, 128:256], xnT[:, c, t0:t1], start=(c == 0), stop=(c == DC - 1))
            sg = sig[k % 2][:, 0:n]
            b.act(sg, pg, AF.Sigmoid, bias=bconv[:, 8 + cp:9 + cp])
            b.stt(h[:, cp, t0:t1], pa, bconv[:, cp:cp + 1], sg, ALU.add, ALU.mult)
            k += 1
        b.tt(h[:, cp, 0:128], h[:, cp, 0:128], halo, ALU.mult)
    s.release()
    b.dbg("h", h, [128, 8, TH], BF16)
    if b.stop_after == "1b":
        return

    acc = s.sb("acc", [128, 8, TOK], F32)
    cw = s.sb("cw_s", [128, 8, CONV_K], F32)
    cvec = s.sb("cvec_s", [128, 3, 8], F32)
    s.dma("sp", cw, cw_d)
    s.dma("sp", cvec, cvec_d)
    dgs = [s.sb("dg%d" % i, [128, CONV_K, 128], BF16) for i in range(2)]
    kc = 0
    for cp in range(8):
        dg = dgs[cp % 2]
        b.tt(dg, ident_b.unsqueeze(1).broadcast_to([128, CONV_K, 128]),
             cw[:, cp, :].unsqueeze(2).broadcast_to([128, CONV_K, 128]), ALU.mult)
        for hf in range(2):
            pc = PS[:, 4 + (kc % 4), :]
            kc += 1
            for kk in range(CONV_K):
                b.mm(pc, dg[:, kk, :], h[:, cp, 98 + kk + hf * 512:98 + kk + hf * 512 + 512], start=(kk == 0), stop=(kk == CONV_K - 1))
            b.act(acc[:, cp, hf * 512:(hf + 1) * 512], pc, AF.Identity, bias=cvec[:, 0, cp:cp + 1])
    b.dbg("acc", acc, [128, 8, TOK])
    s.mark()
    sq = [s.sb("sq%d" % i, [128, 512], F32) for i in range(2)]
    mean = s.sb("mean", [128, 512], F32)
    var = s.sb("var", [128, 512], F32)
    xc = [s.sb("xc%d" % i, [128, 512], F32) for i in range(2)]
    for hf in range(2):
        t0 = hf * 512
        p1 = PS[:, 0, :]
        p2 = PS[:, 1, :]
        for cp in range(8):
            b.mm(p1, onesm, acc[:, cp, t0:t0 + 512], start=(cp == 0), stop=(cp == 7))
        for cp in range(8):
            q_ = sq[cp % 2]
            b.act(q_, acc[:, cp, t0:t0 + 512], AF.Square)
            b.mm(p2, onesm, q_, start=(cp == 0), stop=(cp == 7))
        b.cp(mean, p1, eng="act")
        b.tt(var, mean, mean, ALU.mult)
        b.tt(var, p2, var, ALU.subtract)
        b.ts(var, var, EPS, None, ALU.add)
        b.act(var, var, AF.Sqrt)
        b.recip(var, var)
        for cp in range(8):
            x_ = xc[cp % 2]
            b.tt(x_, acc[:, cp, t0:t0 + 512], mean, ALU.subtract)
            b.tt(x_, x_, var, ALU.mult)
            b.act(mixedT[:, cp, t0:t0 + 512], x_, AF.Silu, bias=cvec[:, 2, cp:cp + 1], scale=cvec[:, 1, cp:cp + 1])
    s.release()
    s.release()
    b.dbg("mixc", mixedT, [128, DC, TOK], BF16)
    if b.stop_after == "1c":
        return

    s.mark()
    qT = s.sb("qT", [128, 8, TOK], BF16)
    kT = s.sb("kT", [128, 2, TH], BF16)
    vx = s.sb("vx", [128, NTH, 2, 65], BF16)
    bqkv = s.sb("bqkv_s", [128, 1280], F32)
    qkg = s.sb("qkg_s", [128, 2, 64], F32)
    esink = s.sb("esink", [128, 16], F32)
    invf = s.sb("invf_s", [128, 8], F32)
    posi = s.sb("posi", [128, NTH], I32)
    posf = s.sb("posf", [128, NTH], F32)
    masks = s.sb("masks_s", [128, 4, 128], BF16)
    s.dma("sp", bqkv, bqkv_d)
    s.dma("sp", qkg, qkg_d)
    s.dma("sp", esink, sink_d)
    s.dma("sp", invf, invf_d)
    s.dma("sp", posi, pos)
    s.dma("pool", masks, masks_d)
    b.cp(posf, posi)
    b.act(esink, esink, AF.Exp)
    b.memset(vx[:, :, :, 64:65], 1.0)
    s.mark()
    qf = s.sb("qf", [128, 1024], F32)
    kvf = s.sb("kvf", [128, 256], F32)
    sqq = s.sb("sqq", [128, 1024], F32)
    qn = s.sb("qn", [128, 1024], F32)
    qb = s.sb("qb", [128, 1024], BF16)
    kn = s.sb("kn", [128, 128], F32)
    kb2 = s.sb("kb2", [128, 2, 2, 64], BF16)
    rq = s.sb("rq", [128, 18], F32)
    ang = s.sb("ang", [128, 2, 8], F32)
    kq = s.sb("kq", [128, 2, 8], F32)
    ki = s.sb("ki", [128, 2, 8], I32)
    sc = s.sb("sc", [128, 2, 8], F32)
    rt = s.sb("rt", [128, 4, 16, 8], F32)
    def trans(j):
        own = j >= 1
        pb = PS[:, 3, :].bitcast(BF16).rearrange("p (a b) -> p a b", a=8)
        if own:
            for pr in range(8):
                b.tr(pb[:, pr, :], qb[:, pr * 128:(pr + 1) * 128], ident_b)
            b.cp(qT[:, :, (j - 1) * 128:j * 128], pb, eng="act")
        pk = PS[:, 4, :].bitcast(BF16).rearrange("p (a b) -> p a b", a=8)
        for g in range(2):
            b.tr(pk[:, g, :], kb2[:, g].rearrange("p a d -> p (a d)"), ident_b)
        b.cp(kT[:, :, j * 128:(j + 1) * 128], pk[:, 0:2, :])

    for j in range(NTH):
        own = j >= 1
        if own:
            for half in range(2):
                pq = PS[:, half, :]
                for c in range(DC):
                    b.mm(pq, xnT[:, c, j * 128:(j + 1) * 128], wq_s[:, c, half * 512:(half + 1) * 512], start=(c == 0), stop=(c == DC - 1))
        pkv = PS[:, 2, 0:256]
        for c in range(DC):
            b.mm(pkv, xnT[:, c, j * 128:(j + 1) * 128], wkv_s[:, c, :], start=(c == 0), stop=(c == DC - 1))
        if own:
            b.tt(qf, PS[:, 0:2, :].rearrange("p a b -> p (a b)"), bqkv[:, 0:1024], ALU.add)
        b.tt(kvf, pkv, bqkv[:, 1024:1280], ALU.add)
        if j > 0:
            trans(j - 1)
        b.ts(ang[:, 0, :], invf, posf[:, j:j + 1], None, ALU.mult)
        b.ts(ang[:, 1, :], ang[:, 0, :], 1.5707963267948966, None, ALU.add)
        b.ts(kq, ang, 1.0 / TWO_PI, None, ALU.mult)
        b.cp(ki, kq)
        b.cp(kq, ki)
        b.stt(ang, kq, -CW1, ang, ALU.mult, ALU.add)
        b.stt(ang, kq, -CW2, ang, ALU.mult, ALU.add)
        b.ts(kq, ang, 3.141592653589793, -TWO_PI, ALU.is_gt, ALU.mult)
        b.tt(ang, ang, kq, ALU.add)
        b.ts(kq, ang, -3.141592653589793, TWO_PI, ALU.is_lt, ALU.mult)
        b.tt(ang, ang, kq, ALU.add)
        b.act(sc, ang, AF.Sin)
        sinb = lambda nh: sc[:, 0, :].unsqueeze(1).broadcast_to([128, nh, 8])
        cosb = lambda nh: sc[:, 1, :].unsqueeze(1).broadcast_to([128, nh, 8])

        def norm_rot(src, nh, gidx, dst_f, dst_b_view, rcol0):
            sv = src.rearrange("p (h d) -> p h d", h=nh)
            b.tt(sqq[:, 0:nh * 64], src, src, ALU.mult)
            b.rsum(rq[:, rcol0:rcol0 + nh], sqq[:, 0:nh * 64].rearrange("p (h d) -> p h d", h=nh))
            b.ts(rq[:, rcol0:rcol0 + nh], rq[:, rcol0:rcol0 + nh], 1.0 / 64, EPS, ALU.mult, ALU.add)
            b.act(rq[:, rcol0:rcol0 + nh], rq[:, rcol0:rcol0 + nh], AF.Sqrt)
            b.recip(rq[:, rcol0:rcol0 + nh], rq[:, rcol0:rcol0 + nh])
            dv = dst_f.rearrange("p (h d) -> p h d", h=nh)
            b.tt(dv, sv, rq[:, rcol0:rcol0 + nh].unsqueeze(2).broadcast_to([128, nh, 64]), ALU.mult)
            b.tt(dv, dv, qkg[:, gidx, :].unsqueeze(1).broadcast_to([128, nh, 64]), ALU.mult)
            b.cp(dst_b_view, dv, eng="act")
            r1 = dv[:, :, 0:8]
            r2 = dv[:, :, 8:16]
            b.tt(rt[:, 0, 0:nh, :], r1, cosb(nh), ALU.mult)
            b.tt(rt[:, 1, 0:nh, :], r2, sinb(nh), ALU.mult)
            b.tt(rt[:, 2, 0:nh, :], r2, cosb(nh), ALU.mult)
            b.tt(rt[:, 3, 0:nh, :], r1, sinb(nh), ALU.mult)
            b.tt(dst_b_view[:, :, 0:8], rt[:, 0, 0:nh, :], rt[:, 1, 0:nh, :], ALU.subtract)
            b.tt(dst_b_view[:, :, 8:16], rt[:, 2, 0:nh, :], rt[:, 3, 0:nh, :], ALU.add)

        if own:
            norm_rot(qf, 16, 0, qn, qb.rearrange("p (h d) -> p h d", h=16), 0)
        norm_rot(kvf[:, 0:128], 2, 1, kn, kb2[:, :, 0, :], 16)
        b.cp(kb2[:, :, 1, :], kb2[:, :, 0, :])
        b.cp(vx[:, j, :, 0:64], kvf[:, 128:256].rearrange("p (g d) -> p g d", g=2), eng="act")
    trans(NTH - 1)
    s.release()
    b.dbg("qT", qT, [128, 8, TOK], BF16)
    b.dbg("kT", kT, [128, 2, TH], BF16)
    b.dbg("vx", vx, [128, NTH, 2, 65], BF16)
    if b.stop_after == "1d":
        return

    s.mark()
    PTs = [s.sb("PT%d" % i, [128, 2, 4, 128], BF16) for i in range(2)]
    attn = [s.sb("attn%d" % i, [128, 16, 64], BF16) for i in range(2)]
    den = s.sb("den", [128, 4, 4], F32)
    WOUT_OFF = 229344 - 3 * DC * 512 * 2
    assert s.sb_off <= WOUT_OFF, s.sb_off
    wout = s.sb_at("wout_s", [128, 3, DC, 512], BF16, WOUT_OFF)
    for i in range(3):
        s.dma("pool", wout[:, i], wout_d[i])
    iters = [(blk, g, hh) for blk in range(NT) for g in range(2) for hh in range(2)]

    def scores(it):
        blk, g, hh = iters[it]
        st = PS[:, (it % 2) * 2:(it % 2) * 2 + 2, :]
        for kb in range(2):
            b.mm(st[:, kb, :], kT[hh * 64:(hh + 1) * 64, g, (blk + kb) * 128:(blk + kb + 1) * 128],
                 qT[hh * 64:(hh + 1) * 64, 4 * g:4 * g + 4, blk * 128:(blk + 1) * 128])

    scores(0)
    for it, (blk, g, hh) in enumerate(iters):
        at = attn[blk % 2]
        st = PS[:, (it % 2) * 2:(it % 2) * 2 + 2, :]
        PT = PTs[it % 2]
        b.act(PT.rearrange("p a h q -> p a (h q)"), st, AF.Exp, scale=0.125)
        if it + 1 < len(iters):
            scores(it + 1)
        mk = masks[:, 0:2, :] if blk == 0 else masks[:, 2:4, :]
        b.tt(PT, PT, mk.unsqueeze(2).broadcast_to([128, 2, 4, 128]), ALU.mult)
        po = PS[:, 4 + (it % 2), 0:260].rearrange("p (h d) -> p h d", h=4)
        for hd in range(4):
            for kb in range(2):
                b.mm(po[:, hd, :], PT[:, kb, hd, :], vx[:, blk + kb, g, :], start=(kb == 0), stop=(kb == 1))
        dn = den[:, it % 4, :]
        h0 = 8 * g + hh
        b.tt(dn, po[:, :, 64], esink[:, h0:h0 + 7:2], ALU.add)
        b.recip(dn, dn)
        b.tt(at[:, h0:h0 + 7:2, :], po[:, :, 0:64], dn.unsqueeze(2).broadcast_to([128, 4, 64]), ALU.mult)
        if g == 1 and hh == 1:
            pb = PS[:, 6 + (blk % 2), :].bitcast(BF16).rearrange("p (a b) -> p a b", a=8)
            for pr in range(8):
                b.tr(pb[:, pr, :], at[:, 2 * pr:2 * pr + 2, :].rearrange("p a d -> p (a d)"), ident_b)
            b.cp(mixedT[:, 8:16, blk * 128:(blk + 1) * 128], pb, eng="act")
    s.release()
    s.release()
    s.release()
    b.dbg("mixedT", mixedT, [128, DC, TOK], BF16)
    if b.stop_after == "1e":
        return
    _emit2(b, locals())


def _emit2(b, L):
    s = b.s
    PS = s.psum
    xin, xnT, mixedT = L["xin"], L["xnT"], L["mixedT"]
    ident_b, ident_f, iota128 = L["ident_b"], L["ident_f"], L["iota128"]
    wout_d, g2bc_d, x1_d, g_d, y_d = L["wout_d"], L["g2bc_d"], L["x1_d"], L["g_d"], L["y_d"]
    wq_d, keys_d, ut_d, v_d, iota_d = L["wq_d"], L["keys_d"], L["ut_d"], L["v_d"], L["iota_d"]
    xn2T = L["xn2T"]
    s.mark()
    wout = L["wout"]
    wout3 = s.sb("wout3_s", [128, DC, 512], BF16)
    s.dma("pool", wout3, wout_d[3])
    g2bc = s.sb("g2bc_s", [128, D], F32)
    ss2 = s.sb("ss2", [128, NT], F32)
    rs2 = s.sb("rs2", [128, NT], F32)
    s.dma("sp", g2bc, g2bc_d)
    xts = [s.sb("xr%d" % i, [128, D], F32) for i in range(2)]
    x1s = [s.sb("x1t%d" % i, [128, D], F32) for i in range(2)]
    xnbs = [s.sb("xn2b%d" % i, [128, D], BF16) for i in range(2)]
    junk = s.sb("junk2", [128, D], BF16)
    assert s.sb_off <= 229344 - 3 * DC * 512 * 2, s.sb_off
    def proj_part(j):
        xt = xts[j % 2]
        x1 = x1s[j % 2]
        s.dma("sp", xt, xin[(j + 1) * 128:(j + 2) * 128, :])
        for qd in range(4):
            pp = PS[:, 4 + qd, :]
            for c in range(DC):
                b.mm(pp, mixedT[:, c, j * 128:(j + 1) * 128], (wout[:, qd, c, :] if qd < 3 else wout3[:, c, :]), start=(c == 0), stop=(c == DC - 1))
            b.tt(x1[:, qd * 512:(qd + 1) * 512], pp, xt[:, qd * 512:(qd + 1) * 512], ALU.add)
        s.dma("sp", x1_d[j * 128:(j + 1) * 128, :], x1)
        b.norm_part(x1, g2bc, ss2[:, j:j + 1], rs2[:, j:j + 1], junk, xnbs[j % 2])

    proj_part(0)
    for j in range(NT):
        if j + 1 < NT:
            proj_part(j + 1)
        b.transpose_part(xnbs[j % 2], xn2T, j * 128, ident_b, (j % 2) * 2)
    s.release()
    s.release()
    b.dbg("xn2T", xn2T, [128, DC, TOK], BF16)
    if b.stop_after == "1f":
        if "x1" in b.debug:
            pass
        return
    _emit3(b, L, xn2T)


def _emit3(b, L, xn2T):
    s = b.s
    PS = s.psum
    ident_f, iota128 = L["ident_f"], L["iota128"]
    x1_d, g_d, y_d = L["x1_d"], L["g_d"], L["y_d"]
    wq_d, keys_d, ut_d, v_d = L["wq_d"], L["keys_d"], L["ut_d"], L["v_d"]
    NEG = -1.0e30
    s.mark()
    v16 = s.sb("v16", [128, NT, 16, 16], F32)
    i16 = s.sb("i16", [128, NT, 16, 16], U32)
    ET = s.sb("ET", [128, 3, TOK], F32)
    E1T = ET[:, 0, :]
    E2T = ET[:, 1, :]
    gT = ET[:, 2, :]
    s.mark()
    keysT = s.sb("keysT_s", [128, 16, 128], BF16)
    s.dma("pool", keysT, keys_d)
    wqs = [s.sb("wqs%d" % i, [128, DC, 128], BF16) for i in range(2)]
    qhs = [s.sb("qhs%d" % i, [128, TOK], BF16) for i in range(2)]
    tmr = [s.sb("tmr%d" % i, [128, 128], F32) for i in range(4)]
    k = 0
    for gi in range(16):
        w = wqs[gi % 2]
        s.dma("pool", w, wq_d[gi])
        qp = PS[:, (gi % 2) * 2:(gi % 2) * 2 + 2, :].rearrange("p a b -> p (a b)")
        for half in range(2):
            for c in range(DC):
                b.mm(qp[:, half * 512:(half + 1) * 512], w[:, c, :], xn2T[:, c, half * 512:(half + 1) * 512], start=(c == 0), stop=(c == DC - 1))
        qh = qhs[gi % 2]
        b.cp(qh, qp, eng="act")
        sp_ = PS[:, 4 + (gi % 2) * 2:6 + (gi % 2) * 2, :].rearrange("p a (t n) -> p (a t) n", n=128)
        for tl in range(NT):
            b.mm(sp_[:, tl, :], qh[:, tl * 128:(tl + 1) * 128], keysT[:, gi, :])
        for tp in range(NT // 4):
            chains = []
            for tl in range(4 * tp, 4 * tp + 4):
                sv = sp_[:, tl, :]
                tm = tmr[tl % 4]
                va = v16[:, tl, gi, 0:8]
                vb = v16[:, tl, gi, 8:16]
                ia = i16[:, tl, gi, 0:8]
                ib = i16[:, tl, gi, 8:16]
                chains.append([
                    (lambda e, va=va, sv=sv: e.max(out=va, in_=sv), [sv], [va]),
                    (lambda e, ia=ia, va=va, sv=sv: e.max_index(out=ia, in_max=va, in_values=sv), [va, sv], [ia]),
                    (lambda e, tm=tm, va=va, sv=sv: e.match_replace(out=tm, in_to_replace=va, in_values=sv, imm_value=NEG), [va, sv], [tm]),
                    (lambda e, vb=vb, tm=tm: e.max(out=vb, in_=tm), [tm], [vb]),
                    (lambda e, ib=ib, vb=vb, tm=tm: e.max_index(out=ib, in_max=vb, in_values=tm), [vb, tm], [ib]),
                ])
            for step in range(5):
                for ch in chains:
                    fn, rd, wr = ch[step]
                    s.op("dve", fn, reads=rd, writes=wr)
    s.release()
    b.dbg("v16", v16, [128, NT, 16, 16])
    b.dbg("i16", i16, [128, NT, 16, 16], U32)
    if b.stop_after == "2a":
        return

    s.mark()
    i16f = s.sb("i16f", [128, 16, 16], F32)
    cand = s.sb("cand", [128, 8, 256], F32)
    cd2 = [s.sb("cd2_%d" % i, [128, 256], F32) for i in range(4)]
    vals = s.sb("vals", [128, 8, 16], F32)
    cidx = s.sb("cidx", [128, 8, 16], U32)
    aku = s.sb("aku", [128, 128], U32)
    bku = s.sb("bku", [128, 128], U32)
    akf = s.sb("akf", [128, 128], F32)
    bkf = s.sb("bkf", [128, 128], F32)
    eq = s.sb("eq", [128, 128, 16], F32)
    E12 = s.sb("E12", [128, 3, 128], F32)
    dd = s.sb("dd", [128, 8, 16], F32)
    zz = s.sb("zz", [128, 8], F32)
    io16 = iota128[:, 0:16].unsqueeze(1).broadcast_to([128, 128, 16])
    NOH = 16
    oh1 = [s.sb("oh1_%d" % i, [128, 128], BF16) for i in range(NOH)]
    oh2 = [s.sb("oh2_%d" % i, [128, 128], BF16) for i in range(NOH)]
    gst = [s.sb("gst%d" % i, [128, 128, 128], BF16) for i in range(2)]
    iota_b = s.sb("iota_b", [128, 128], BF16)
    b.cp(iota_b, iota128)

    def gen_2b(tl):
        b.cp(i16f, i16[:, tl])
        yield
        vv = v16[:, tl].rearrange("p (h two) k -> p h two k", two=2)
        in0 = vv[:, :, 0, :].unsqueeze(3).broadcast_to([128, 8, 16, 16])
        in1 = vv[:, :, 1, :].unsqueeze(2).broadcast_to([128, 8, 16, 16])
        b.tt(cand.rearrange("p h (a c) -> p h a c", a=16), in0, in1, ALU.add)
        yield
        for hp in range(2):
            chains = []
            for hh in range(4 * hp, 4 * hp + 4):
                cv = cand[:, hh, :]
                c2 = cd2[hh % 4]
                va = vals[:, hh, 0:8]
                vb = vals[:, hh, 8:16]
                ia = cidx[:, hh, 0:8]
                ib = cidx[:, hh, 8:16]
                chains.append([
                    (lambda e, va=va, cv=cv: e.max(out=va, in_=cv), [cv], [va]),
                    (lambda e, ia=ia, va=va, cv=cv: e.max_index(out=ia, in_max=va, in_values=cv), [va, cv], [ia]),
                    (lambda e, c2=c2, va=va, cv=cv: e.match_replace(out=c2, in_to_replace=va, in_values=cv, imm_value=NEG), [va, cv], [c2]),
                    (lambda e, vb=vb, c2=c2: e.max(out=vb, in_=c2), [c2], [vb]),
                    (lambda e, ib=ib, vb=vb, c2=c2: e.max_index(out=ib, in_max=vb, in_values=c2), [vb, c2], [ib]),
                ])
            for step in range(5):
                for ch in chains:
                    fn, rd, wr = ch[step]
                    s.op("dve", fn, reads=rd, writes=wr)
                    yield
        cf = cidx.rearrange("p h k -> p (h k)")
        s.op("dve", lambda e, cf=cf: e.tensor_single_scalar(out=aku, in_=cf, scalar=4, op=ALU.logical_shift_right), reads=[cf], writes=[aku])
        yield
        s.op("dve", lambda e, cf=cf: e.tensor_single_scalar(out=bku, in_=cf, scalar=15, op=ALU.bitwise_and), reads=[cf], writes=[bku])
        yield
        b.cp(akf, aku)
        yield
        b.cp(bkf, bku)
        yield
        i16v = i16f.rearrange("p (h two) k -> p h two k", two=2)
        for which, (kf, par) in enumerate(((akf, 0), (bkf, 1))):
            b.tt(eq, kf.unsqueeze(2).broadcast_to([128, 128, 16]), io16, ALU.is_equal)
            yield
            e4 = eq.rearrange("p (h k) a -> p h k a", h=8)
            b.tt(e4, e4, i16v[:, :, par, :].unsqueeze(2).broadcast_to([128, 8, 16, 16]), ALU.mult)
            yield
            b.rsum(E12[:, which, :], eq)
            yield
        b.tt(dd, vals, vals[:, :, 0:1].broadcast_to([128, 8, 16]), ALU.subtract)
        yield
        b.act(dd, dd, AF.Exp)
        b.rsum(zz, dd)
        yield
        b.recip(zz, zz)
        yield
        b.tt(E12[:, 2, :].rearrange("p (h k) -> p h k", h=8), dd, zz.unsqueeze(2).broadcast_to([128, 8, 16]), ALU.mult)
        yield
        pt = PS[:, tl % 2, 0:384].rearrange("p (a b) -> p a b", a=3)
        for i in range(3):
            b.tr(pt[:, i, :], E12[:, i, :], ident_f)
        b.cp(ET[:, :, tl * 128:(tl + 1) * 128], pt, eng="act")
        yield

    UT_OFF = 229344 - 8 * DC * 128 * 2
    assert s.sb_off <= UT_OFF, s.sb_off
    uts = [s.sb_at("ut%d" % i, [128, DC, 128], BF16, UT_OFF + i * DC * 128 * 2) for i in range(8)]
    for c in range(8):
        s.dma("pool", uts[c], ut_d[c])
    for _ in gen_2b(0):
        pass
    for tl in range(NT):
        G = gst[tl % 2]
        nxt = gen_2b(tl + 1) if tl + 1 < NT else None
        for t8 in range(16):
            gp = PS[:, 4 + (t8 % 2) * 2:6 + (t8 % 2) * 2, :].rearrange("p a (t e) -> p (a t) e", e=128)
            for ti in range(8):
                tok = t8 * 8 + ti
                t = tl * 128 + tok
                o1 = oh1[tok % NOH]
                o2 = oh2[tok % NOH]
                b.ts(o1, iota_b, E1T[:, t:t + 1], None, ALU.is_equal)
                b.ts(o2, iota_b, E2T[:, t:t + 1], gT[:, t:t + 1], ALU.is_equal, ALU.mult)
                b.mm(gp[:, ti, :], o2, o1)
                if nxt is not None and tok % 2 == 1:
                    next(nxt, None)
            b.cp(G[:, :, t8 * 8:(t8 + 1) * 8], gp.rearrange("p t e -> p e t"), eng="act")
        if nxt is not None:
            for _ in nxt:
                pass
        for q4 in range(4):
            s.dma("sp", g_d[q4 * 32:(q4 + 1) * 32, :, tl * 128:(tl + 1) * 128].rearrange("c e t -> e c t"), G[:, q4 * 32:(q4 + 1) * 32, :])
    s.release()
    b.dbg("E1T", E1T, [128, TOK])
    b.dbg("E2T", E2T, [128, TOK])
    b.dbg("gT", gT, [128, TOK])
    s.release()
    if b.stop_after == "2c":
        return

    yacc = s.sb("yacc", [128, NT, D], F32)
    NCH = 128
    CG = 4
    NG = NCH // CG
    assert 2 * CG == 8
    vgs = [s.sb("vg%d" % i, [128, CG, D], BF16) for i in range(2)]
    gts = [s.sb("gt%d" % i, [128, TOK], BF16) for i in range(2 * CG)]
    gls = [s.sb("gl%d" % i, [128, TOK], F32) for i in range(2)]
    aTs = [s.sb("aT%d" % i, [128, CG, TOK], BF16) for i in range(2)]
    assert s.sb_off <= UT_OFF, s.sb_off

    def load_v(g):
        for ci in range(CG):
            c = g * CG + ci
            s.dma("pool", vgs[g % 2][:, ci, :], v_d[c * 128:(c + 1) * 128, :])

    def load_u(g):
        for ci in range(CG):
            c = g * CG + ci
            if g >= 2:
                s.dma("pool", uts[c % (2 * CG)], ut_d[c])
            s.dma("sp", gts[c % (2 * CG)], g_d[c])

    def h_group(g):
        for ci in range(CG):
            c = g * CG + ci
            u = uts[c % (2 * CG)]
            hp = PS[:, (c % 2) * 2:(c % 2) * 2 + 2, :].rearrange("p a b -> p (a b)")
            for half in range(2):
                for dc in range(DC):
                    b.mm(hp[:, half * 512:(half + 1) * 512], u[:, dc, :], xn2T[:, dc, half * 512:(half + 1) * 512], start=(dc == 0), stop=(dc == DC - 1))
            gl = gls[c % 2]
            b.act(gl, hp, AF.Gelu)
            b.tt(aTs[g % 2][:, ci, :], gl, gts[c % (2 * CG)], ALU.mult)

    ycnt = [0]

    def y_group(g):
        a = aTs[g % 2]
        vg = vgs[g % 2]
        for tl in range(NT):
            for qd in range(4):
                yp = PS[:, 4 + (ycnt[0] % 4), :]
                ycnt[0] += 1
                for ci in range(CG):
                    b.mm(yp, a[:, ci, tl * 128:(tl + 1) * 128], vg[:, ci, qd * 512:(qd + 1) * 512], start=(ci == 0), stop=(ci == CG - 1))
                ya = yacc[:, tl, qd * 512:(qd + 1) * 512]
                b.tt(ya, ya, yp, ALU.add)

    ng = NG if b.stop_after != "3s" else 2
    load_u(0)
    if ng > 1:
        load_u(1)
    load_v(0)
    for j in range(NT):
        s.dma("sp", yacc[:, j, :], x1_d[j * 128:(j + 1) * 128, :])
    if ng > 1:
        load_v(1)
    for g in range(ng):
        h_group(g)
        if g >= 1:
            y_group(g - 1)
        if g + 1 < ng and g >= 1:
            load_v(g + 1)
        if g + 2 < ng:
            load_u(g + 2)
    y_group(ng - 1)
    for j in range(NT):
        s.dma("sp", y_d[j * 128:(j + 1) * 128, :], yacc[:, j, :], is_output=True)


def _bc(v, n=128):
    return np.ascontiguousarray(np.broadcast_to(np.asarray(v, np.float32).reshape(1, -1), (n, v.size)))


def prepare_inputs(x, positions, norm1_g, w_in, b_in, conv_dw_w, conv_dw_b, conv_ln_g, conv_ln_b,
                   q_norm_g, k_norm_g, attn_sinks, w_out, norm2_g, peer_w_q, peer_sub_keys,
                   peer_u, peer_v):
    f32 = np.float32
    x = np.asarray(x, f32).reshape(SEQ, D)
    positions = np.asarray(positions).reshape(SEQ).astype(np.int32)
    w_in = np.asarray(w_in, f32)
    b_in = np.asarray(b_in, f32)
    shared = {}
    shared["g1bc"] = _bc(norm1_g)
    shared["g2bc"] = _bc(norm2_g)
    w_l = w_in.reshape(DC, 128, 3328).transpose(1, 0, 2)
    wconv = np.empty((8, 128, DC, 256), f32)
    for cp in range(8):
        wconv[cp, :, :, 0:128] = w_l[:, :, cp * 128:(cp + 1) * 128]
        wconv[cp, :, :, 128:256] = w_l[:, :, 1024 + cp * 128:1024 + (cp + 1) * 128]
    shared["wconv"] = wconv
    shared["wqkv"] = np.ascontiguousarray(w_l[:, :, 2048:3328])
    shared["bconv"] = np.ascontiguousarray(b_in[0:2048].reshape(16, 128).T)
    shared["bqkvbc"] = _bc(b_in[2048:3328])
    shared["convw"] = np.ascontiguousarray(np.asarray(conv_dw_w, f32).reshape(CONV_K, 8, 128).transpose(2, 1, 0))
    cv = np.stack([np.asarray(a, f32).reshape(8, 128).T for a in (conv_dw_b, conv_ln_g, conv_ln_b)], axis=1)
    shared["convvec"] = np.ascontiguousarray(cv)
    shared["qkgbc"] = np.ascontiguousarray(np.stack([_bc(q_norm_g), _bc(k_norm_g)], axis=1))
    shared["sinkbc"] = _bc(attn_sinks)
    inv_freq = (500000.0 ** (-np.arange(0, 16, 2, dtype=np.float32) / 16)).astype(f32)
    shared["invfbc"] = _bc(inv_freq)
    shared["wout"] = np.ascontiguousarray(np.asarray(w_out, f32).reshape(DC, 128, 4, 512).transpose(2, 1, 0, 3))
    wq_l = np.asarray(peer_w_q, f32).reshape(DC, 128, 16, 128).transpose(2, 1, 0, 3)
    shared["wq"] = np.ascontiguousarray(wq_l)
    keys = np.asarray(peer_sub_keys, f32).reshape(16, 128, 128)
    shared["keysT"] = np.ascontiguousarray(keys.transpose(2, 0, 1))
    u = np.asarray(peer_u, f32).reshape(128, 128, DC, 128)
    shared["ut"] = np.ascontiguousarray(u.transpose(0, 3, 2, 1))
    shared["pv"] = np.ascontiguousarray(np.asarray(peer_v, f32))
    io = np.empty((128, 128 + 2048), f32)
    io[:, 0:128] = np.arange(128, dtype=f32)[None, :]
    io[:, 128:] = (np.arange(2048) % 16).astype(f32)[None, :]
    shared["iotas"] = io
    si = np.arange(128)[:, None]
    qi = np.arange(128)[None, :]
    mprev = (si > qi).astype(f32)
    mcur = (si <= qi).astype(f32)
    in_maps = []
    for c in range(NCORES):
        m = dict(shared)
        t0 = c * TOK
        xin = np.zeros((TH, D), f32)
        xin[128:] = x[t0:t0 + TOK]
        pp = np.zeros((TH,), np.int32)
        pp[128:] = positions[t0:t0 + TOK]
        if c > 0:
            xin[:128] = x[t0 - 128:t0]
            pp[:128] = positions[t0 - 128:t0]
        m["xin"] = xin
        m["pos"] = np.ascontiguousarray(pp.reshape(NTH, 128).T)
        first = mprev if c > 0 else np.zeros_like(mprev)
        m["masks"] = np.ascontiguousarray(np.stack([first, mcur, mprev, mcur], axis=1))
        m["halomask"] = np.full((128, 128), 1.0 if c > 0 else 0.0, f32)
        in_maps.append(m)
    return in_maps


_PROGRAM = None


def kernel(**inputs):
    global _PROGRAM
    in_maps = prepare_inputs(**inputs)
    if _PROGRAM is None:
        _PROGRAM = build_program()[0]
    res = run_bass_kernel_spmd(_PROGRAM, in_maps, core_ids=list(range(NCORES)))
    out = np.concatenate([r["y"] for r in res.results], axis=0)
    return out.reshape(1, SEQ, D).astype(np.float32)
```

```python
import contextlib
import numpy as np
import concourse.bass as bass
import concourse.mybir as mybir
from concourse.bass_utils import run_bass_kernel_spmd

F32 = mybir.dt.float32
BF16 = mybir.dt.bfloat16
U32 = mybir.dt.uint32
I32 = mybir.dt.int32
AF = mybir.ActivationFunctionType
ALU = mybir.AluOpType
AX = mybir.AxisListType

DSIZE = {F32: 4, BF16: 2, U32: 4, I32: 4, mybir.dt.uint16: 2, mybir.dt.uint8: 1}


class Sched:
    ENGS = ("pe", "act", "dve", "pool", "sp")

    def __init__(self, nc, stack, same_engine_sync=True, n_dma_sems=24):
        self.nc = nc
        self.eng_obj = {"pe": nc.tensor, "act": nc.scalar, "dve": nc.vector,
                        "pool": nc.gpsimd, "sp": nc.sync}
        self.prog = {e: [] for e in self.ENGS}
        self.sem = {e: stack.enter_context(nc.semaphore("s_" + e)) for e in self.ENGS}
        self.cnt = {e: 0 for e in self.ENGS}
        self.seen = {e: {} for e in self.ENGS}
        self.same_engine_sync = same_engine_sync
        self.dma_sems = [stack.enter_context(nc.semaphore("d%d" % i)) for i in range(2 * n_dma_sems)]
        self.dma_val = [0] * (2 * n_dma_sems)
        self.dma_rr = {"sp": 0, "pool": 0}
        self.n_dma_sems = n_dma_sems
        self.semkey = {}
        for e in self.ENGS:
            self.semkey[("e", e)] = self.sem[e]
        for i, s in enumerate(self.dma_sems):
            self.semkey[("d", i)] = s
        self.records = {"sb": [], "ps": [], "dr": []}
        self.tinfo = {}
        self.sb_off = 16640
        self.sb_stack = []
        pt = nc.alloc_psum_tensor("psum_all", [128, 8, 512], F32)
        self.psum = pt.ap()
        self.tinfo[pt.name] = ("ps", 0, 4)
        self.n_inst = 0
        self.out_tokens = []

    def sb(self, name, shape, dtype, align=64):
        nbytes = int(np.prod(shape[1:])) * DSIZE[dtype]
        off = (self.sb_off + align - 1) // align * align
        t = self.nc.alloc_sbuf_tensor_at(name, list(shape), dtype, offset=off)
        self.sb_off = off + nbytes
        assert self.sb_off <= 229344, ("SBUF overflow", name, self.sb_off)
        self.tinfo[t.name] = ("sb", off, DSIZE[dtype])
        return t.ap()

    def sb_at(self, name, shape, dtype, off):
        t = self.nc.alloc_sbuf_tensor_at(name, list(shape), dtype, offset=off)
        self.tinfo[t.name] = ("sb", off, DSIZE[dtype])
        return t.ap()

    def mark(self):
        self.sb_stack.append(self.sb_off)

    def release(self):
        self.sb_off = self.sb_stack.pop()

    def dram(self, name, shape, dtype, kind):
        t = self.nc.dram_tensor(name, list(shape), dtype, kind=kind)
        self.tinfo[t.name] = ("dr", 0, DSIZE[dtype])
        return t.ap()

    def reg_tensor(self, name, space, base, dsize):
        self.tinfo[name] = (space, base, dsize)

    def region(self, ap):
        name = ap.tensor.name
        space, base, dsz = self.tinfo[name]
        dsz = DSIZE[ap.dtype]
        dims = [tuple(d) for d in ap.ap]
        if space == "dr":
            off = ap.offset
            lo = off
            hi = off + sum((c - 1) * abs(s) for s, c in dims) + 1
            return (name, lo * dsz, hi * dsz)
        pstride = dims[0][0]
        off = ap.offset
        if pstride > 0:
            off = off % pstride
        lo = off
        hi = off + sum((c - 1) * abs(s) for s, c in dims[1:]) + 1
        lo_b = base + lo * dsz
        hi_b = base + hi * dsz
        if space == "ps":
            lo_b = lo_b // 2048 * 2048
            hi_b = (hi_b + 2047) // 2048 * 2048
        return (space, lo_b, hi_b)

    def _deps(self, reads, writes):
        deps = {}
        def add(tok):
            k, v = tok
            if deps.get(k, -1) < v:
                deps[k] = v
        rregs = [self.region(a) for a in reads]
        wregs = [self.region(a) for a in writes]
        for (sp, lo, hi) in rregs:
            for rec in self.records_for(sp):
                if rec[3] and rec[0] == sp and rec[1] < hi and lo < rec[2]:
                    add(rec[4])
        for (sp, lo, hi) in wregs:
            for rec in self.records_for(sp):
                if rec[0] == sp and rec[1] < hi and lo < rec[2]:
                    add(rec[4])
        return deps, rregs, wregs

    def records_for(self, sp):
        if sp in ("sb", "ps"):
            return self.records[sp]
        return self.records["dr"]

    def _commit(self, rregs, wregs, tok):
        for (sp, lo, hi) in wregs:
            lst = self.records_for(sp)
            lst[:] = [r for r in lst if not (r[0] == sp and lo <= r[1] and r[2] <= hi)]
            lst.append((sp, lo, hi, True, tok))
        for (sp, lo, hi) in rregs:
            lst = self.records_for(sp)
            for i, r in enumerate(lst):
                if (not r[3]) and r[0] == sp and r[1] == lo and r[2] == hi and r[4][0] == tok[0]:
                    lst[i] = (sp, lo, hi, False, tok)
                    break
            else:
                lst.append((sp, lo, hi, False, tok))

    def _emit_waits(self, eng, deps):
        for k, v in deps.items():
            if k == ("e", eng):
                if eng == "pe" or not self.same_engine_sync:
                    continue
            if self.seen[eng].get(k, 0) >= v:
                continue
            self.seen[eng][k] = v
            self.prog[eng].append(("wait", k, v))

    def op(self, eng, fn, reads=(), writes=(), extra_deps=()):
        deps, rregs, wregs = self._deps(reads, writes)
        for tok in extra_deps:
            if deps.get(tok[0], -1) < tok[1]:
                deps[tok[0]] = tok[1]
        self._emit_waits(eng, deps)
        self.cnt[eng] += 1
        tok = (("e", eng), self.cnt[eng])
        self.prog[eng].append(("inst", fn, ("e", eng), 1))
        self._commit(rregs, wregs, tok)
        self.n_inst += 1
        return tok

    def dma(self, eng, out, in_, is_output=False, **kw):
        deps, rregs, wregs = self._deps([in_], [out])
        qn = "pool" if eng == "pool" else "sp"
        i = self.dma_rr[qn] + (self.n_dma_sems if qn == "pool" else 0)
        self.dma_rr[qn] = (self.dma_rr[qn] + 1) % self.n_dma_sems
        k = ("d", i)
        if self.dma_val[i] > 0:
            if deps.get(k, -1) < self.dma_val[i]:
                deps[k] = self.dma_val[i]
        self._emit_waits(eng, deps)
        self.dma_val[i] += 16
        tok = (k, self.dma_val[i])

        def fn(e, out=out, in_=in_, kw=kw):
            return e.dma_start(out=out, in_=in_, **kw)
        self.prog[eng].append(("inst", fn, k, 16))
        self._commit(rregs, wregs, tok)
        self.n_inst += 1
        if is_output:
            self.out_tokens.append(tok)
        return tok

    def finish(self):
        deps = {}
        for k, v in self.out_tokens:
            if deps.get(k, -1) < v:
                deps[k] = v
        self._emit_waits("sp", deps)

    def replay(self, block):
        nc = self.nc
        semkey = self.semkey

        def mk(engname):
            def body(e):
                for item in self.prog[engname]:
                    if item[0] == "wait":
                        e.wait_ge(semkey[item[1]], item[2])
                    else:
                        inst = item[1](e)
                        inst.then_inc(semkey[item[2]], item[3])
            return body
        block.tensor(mk("pe"))
        block.scalar(mk("act"))
        block.vector(mk("dve"))
        block.gpsimd(mk("pool"))
        block.sync(mk("sp"))


NCORES = 8
D = 2048
SEQ = 8192
TOK = SEQ // NCORES
NT = TOK // 128
NTH = NT + 1
TH = TOK + 128
DC = D // 128
CONV_CH = 1024
CONV_K = 31
EPS = 1e-6
NKEYS = 128
PH = 8
TWO_PI = 6.283185307179586
CW1 = 6.28125
CW2 = TWO_PI - CW1


class Builder:
    def __init__(self, nc, stack, debug=None, stop_after=None):
        self.nc = nc
        self.s = Sched(nc, stack)
        self.debug = debug or []
        self.stop_after = stop_after
        self.dbg_outs = {}

    def mm(self, out, lhsT, rhs, start=True, stop=True):
        return self.s.op("pe", lambda e: e.matmul(out, lhsT=lhsT, rhs=rhs, start=start, stop=stop),
                         reads=[lhsT, rhs] + ([] if start else [out]), writes=[out])

    def tr(self, out, in_, ident):
        return self.s.op("pe", lambda e: e.transpose(out, in_, ident), reads=[in_, ident], writes=[out])

    def act(self, out, in_, func, bias=None, scale=None, accum=None, eng="act"):
        kw = {}
        reads = [in_]
        if bias is not None:
            kw["bias"] = bias
            if not isinstance(bias, (int, float)):
                reads.append(bias)
        if scale is not None:
            kw["scale"] = scale
            if not isinstance(scale, (int, float)):
                reads.append(scale)
        writes = [out]
        if accum is not None:
            kw["accum_out"] = accum
            writes.append(accum)
        return self.s.op(eng, lambda e: e.activation(out=out, in_=in_, func=func, **kw), reads=reads, writes=writes)

    def tt(self, out, in0, in1, op, eng="dve"):
        return self.s.op(eng, lambda e: e.tensor_tensor(out=out, in0=in0, in1=in1, op=op), reads=[in0, in1], writes=[out])

    def ts(self, out, in0, s1, s2, op0, op1=None, eng="dve"):
        reads = [in0]
        for x in (s1, s2):
            if x is not None and not isinstance(x, (int, float)):
                reads.append(x)
        if op1 is None:
            return self.s.op(eng, lambda e: e.tensor_scalar(out=out, in0=in0, scalar1=s1, scalar2=None, op0=op0), reads=reads, writes=[out])
        return self.s.op(eng, lambda e: e.tensor_scalar(out=out, in0=in0, scalar1=s1, scalar2=s2, op0=op0, op1=op1), reads=reads, writes=[out])

    def stt(self, out, in0, scalar, in1, op0, op1):
        reads = [in0, in1]
        if not isinstance(scalar, (int, float)):
            reads.append(scalar)
        return self.s.op("dve", lambda e: e.scalar_tensor_tensor(out=out, in0=in0, scalar=scalar, in1=in1, op0=op0, op1=op1), reads=reads, writes=[out])

    def cp(self, out, in_, eng="dve"):
        if eng == "act":
            return self.s.op("act", lambda e: e.copy(out=out, in_=in_), reads=[in_], writes=[out])
        return self.s.op(eng, lambda e: e.tensor_copy(out=out, in_=in_), reads=[in_], writes=[out])

    def memset(self, ap, val, eng="dve"):
        return self.s.op(eng, lambda e: e.memset(ap, val), writes=[ap])

    def recip(self, out, in_):
        return self.s.op("dve", lambda e: e.reciprocal(out=out, in_=in_), reads=[in_], writes=[out])

    def rsum(self, out, in_):
        return self.s.op("dve", lambda e: e.reduce_sum(out=out, in_=in_, axis=AX.X), reads=[in_], writes=[out])

    def dbg(self, name, ap_sb, shape, dtype=F32):
        if name not in self.debug:
            return
        d = self.s.dram("dbg_" + name, list(shape), dtype, "ExternalOutput")
        self.s.dma("sp", d, ap_sb, is_output=True)
        self.dbg_outs[name] = "dbg_" + name

    def norm_part(self, xt, gbc, ss_col, rstd_col, junk, xnb):
        self.act(junk, xt, AF.Square, accum=ss_col)
        self.ts(rstd_col, ss_col, 1.0 / D, EPS, ALU.mult, ALU.add)
        self.act(rstd_col, rstd_col, AF.Sqrt)
        self.recip(rstd_col, rstd_col)
        self.stt(xnb, xt, rstd_col, gbc, ALU.mult, ALU.mult)

    def transpose_part(self, xnb, dstT, col0, ident_bf, psb):
        s = self.s
        for half in range(2):
            pb = s.psum[:, psb + half, :].bitcast(BF16).rearrange("p (a b) -> p a b", a=8)
            for i in range(8):
                c = half * 8 + i
                self.tr(pb[:, i, :], xnb[:, c * 128:(c + 1) * 128], ident_bf)
            self.cp(dstT[:, half * 8:(half + 1) * 8, col0:col0 + 128], pb, eng="act" if half == 0 else "dve")


def build_program(debug=None, stop_after=None):
    nc = bass.Bass("TRN2", target_bir_lowering=False)
    with contextlib.ExitStack() as stack:
        b = Builder(nc, stack, debug=debug, stop_after=stop_after)
        _emit(b)
        b.s.finish()
        with nc.Block() as block:
            b.s.replay(block)
    return nc, b


def _emit(b):
    s = b.s
    nc = b.nc
    dr = lambda name, shape, dt=F32: s.dram(name, shape, dt, "ExternalInput")
    xin = dr("xin", [TH, D])
    pos = dr("pos", [128, NTH], I32)
    g1bc_d = dr("g1bc", [128, D])
    g2bc_d = dr("g2bc", [128, D])
    wconv_d = dr("wconv", [8, 128, DC, 256])
    wqkv_d = dr("wqkv", [128, DC, 1280])
    bconv_d = dr("bconv", [128, 16])
    bqkv_d = dr("bqkvbc", [128, 1280])
    cw_d = dr("convw", [128, 8, CONV_K])
    cvec_d = dr("convvec", [128, 3, 8])
    qkg_d = dr("qkgbc", [128, 2, 64])
    sink_d = dr("sinkbc", [128, 16])
    invf_d = dr("invfbc", [128, 8])
    masks_d = dr("masks", [128, 4, 128])
    halo_d = dr("halomask", [128, 128])
    wout_d = dr("wout", [4, 128, DC, 512])
    wq_d = dr("wq", [16, 128, DC, 128])
    keys_d = dr("keysT", [128, 16, 128])
    ut_d = dr("ut", [128, 128, DC, 128])
    v_d = dr("pv", [128 * 128, D])
    iota_d = dr("iotas", [128, 128 + 2048])
    y_d = s.dram("y", [TOK, D], F32, "ExternalOutput")
    x1_d = s.dram("x1scratch", [TOK, D], F32, "Internal")
    g_d = s.dram("gscratch", [128, 128, TOK], BF16, "Internal")

    ident_f = s.sb("ident_f", [128, 128], F32)
    ident_b = s.sb("ident_b", [128, 128], BF16)
    iota128 = s.sb("iota128", [128, 128], F32)
    onesm = s.sb("onesm", [128, 128], F32)
    small = s.sb("small", [128, 64], F32)
    s.dma("sp", iota128, iota_d[:, 0:128])
    pidx = s.sb("pidx", [128, 1], F32)
    s.op("pool", lambda e: e.iota(pidx, pattern=[[0, 1]], base=0, channel_multiplier=1,
                                  allow_small_or_imprecise_dtypes=True), writes=[pidx])
    b.ts(ident_f, iota128, pidx, None, ALU.is_equal)
    b.cp(ident_b, ident_f)
    b.memset(onesm, 1.0 / CONV_CH)

    PS = s.psum
    r0 = (s.sb_off + 63) // 64 * 64
    wq_s = s.sb("wq_s", [128, DC, 1024], BF16)
    xn2T = s.sb_at("xn2T", [128, DC, TOK], BF16, r0)
    s.mark()
    mixedT = s.sb("mixedT", [128, DC, TOK], BF16)
    s.mark()
    xnT = s.sb("xnT", [128, DC, TH], BF16)
    wkv_s = s.sb("wkv_s", [128, DC, 256], BF16)
    g1bc = s.sb("g1bc_s", [128, D], F32)
    s.dma("sp", g1bc, g1bc_d)
    ss1 = s.sb("ss1", [128, NTH], F32)
    rs1 = s.sb("rs1", [128, NTH], F32)
    s.mark()
    xts = [s.sb("xt%d" % i, [128, D], F32) for i in range(4)]
    xnbs = [s.sb("xnb%d" % i, [128, D], BF16) for i in range(2)]
    junk = s.sb("junk", [128, D], BF16)
    def part_a(j):
        xt = xts[j % 4]
        s.dma("sp", xt, xin[j * 128:(j + 1) * 128, :])
        b.norm_part(xt, g1bc, ss1[:, j:j + 1], rs1[:, j:j + 1], junk, xnbs[j % 2])

    part_a(0)
    for j in range(NTH):
        if j + 1 < NTH:
            part_a(j + 1)
        b.transpose_part(xnbs[j % 2], xnT, j * 128, ident_b, (j % 2) * 2)
    s.release()
    b.dbg("xnT", xnT, [128, DC, TH], BF16)
    if b.stop_after == "1a":
        return

    s.mark()
    h = s.sb("h", [128, 8, TH], BF16)
    bconv = s.sb("bconv_s", [128, 16], F32)
    s.dma("sp", bconv, bconv_d)
    halo = s.sb("halo_s", [128, 128], BF16)
    s.dma("pool", halo, halo_d)
    s.mark()
    wcb = [s.sb("wcb%d" % i, [128, DC, 256], BF16) for i in range(2)]
    sig = [s.sb("sig%d" % i, [128, 512], F32) for i in range(2)]
    TR = [(0, 512), (512, 1024), (1024, TH)]
    k = 0
    for cp in range(8):
        w = wcb[cp % 2]
        s.dma("pool", w, wconv_d[cp])
        if cp == 7:
            for i in range(4):
                s.dma("pool", wq_s[:, :, i * 256:(i + 1) * 256], wqkv_d[:, :, i * 256:(i + 1) * 256])
            s.dma("pool", wkv_s, wqkv_d[:, :, 1024:1280])
        for (t0, t1) in TR:
            n = t1 - t0
            pa = PS[:, (k % 2) * 2, 0:n]
            pg = PS[:, (k % 2) * 2 + 1, 0:n]
            for c in range(DC):
                b.mm(pa, w[:, c, 0:128], xnT[:, c, t0:t1], start=(c == 0), stop=(c == DC - 1))
            for c in range(DC):
                b.mm(pg, w[:, c, 128:256], xnT[:, c, t0:t1], start=(c == 0), stop=(c == DC - 1))
            sg = sig[k % 2][:, 0:n]
            b.act(sg, pg, AF.Sigmoid, bias=bconv[:, 8 + cp:9 + cp])
            b.stt(h[:, cp, t0:t1], pa, bconv[:, cp:cp + 1], sg, ALU.add, ALU.mult)
            k += 1
        b.tt(h[:, cp, 0:128], h[:, cp, 0:128], halo, ALU.mult)
    s.release()
    b.dbg("h", h, [128, 8, TH], BF16)
    if b.stop_after == "1b":
        return

    acc = s.sb("acc", [128, 8, TOK], F32)
    cw = s.sb("cw_s", [128, 8, CONV_K], F32)
    cvec = s.sb("cvec_s", [128, 3, 8], F32)
    s.dma("sp", cw, cw_d)
    s.dma("sp", cvec, cvec_d)
    dgs = [s.sb("dg%d" % i, [128, CONV_K, 128], BF16) for i in range(2)]
    kc = 0
    for cp in range(8):
        dg = dgs[cp % 2]
        b.tt(dg, ident_b.unsqueeze(1).broadcast_to([128, CONV_K, 128]),
             cw[:, cp, :].unsqueeze(2).broadcast_to([128, CONV_K, 128]), ALU.mult)
        for hf in range(2):
            pc = PS[:, 4 + (kc % 4), :]
            kc += 1
            for kk in range(CONV_K):
                b.mm(pc, dg[:, kk, :], h[:, cp, 98 + kk + hf * 512:98 + kk + hf * 512 + 512], start=(kk == 0), stop=(kk == CONV_K - 1))
            b.act(acc[:, cp, hf * 512:(hf + 1) * 512], pc, AF.Identity, bias=cvec[:, 0, cp:cp + 1])
    b.dbg("acc", acc, [128, 8, TOK])
    s.mark()
    sq = [s.sb("sq%d" % i, [128, 512], F32) for i in range(2)]
    mean = s.sb("mean", [128, 512], F32)
    var = s.sb("var", [128, 512], F32)
    xc = [s.sb("xc%d" % i, [128, 512], F32) for i in range(2)]
    for hf in range(2):
        t0 = hf * 512
        p1 = PS[:, 0, :]
        p2 = PS[:, 1, :]
        for cp in range(8):
            b.mm(p1, onesm, acc[:, cp, t0:t0 + 512], start=(cp == 0), stop=(cp == 7))
        for cp in range(8):
            q_ = sq[cp % 2]
            b.act(q_, acc[:, cp, t0:t0 + 512], AF.Square)
            b.mm(p2, onesm, q_, start=(cp == 0), stop=(cp == 7))
        b.cp(mean, p1, eng="act")
        b.tt(var, mean, mean, ALU.mult)
        b.tt(var, p2, var, ALU.subtract)
        b.ts(var, var, EPS, None, ALU.add)
        b.act(var, var, AF.Sqrt)
        b.recip(var, var)
        for cp in range(8):
            x_ = xc[cp % 2]
            b.tt(x_, acc[:, cp, t0:t0 + 512], mean, ALU.subtract)
            b.tt(x_, x_, var, ALU.mult)
            b.act(mixedT[:, cp, t0:t0 + 512], x_, AF.Silu, bias=cvec[:, 2, cp:cp + 1], scale=cvec[:, 1, cp:cp + 1])
    s.release()
    s.release()
    b.dbg("mixc", mixedT, [128, DC, TOK], BF16)
    if b.stop_after == "1c":
        return

    s.mark()
    qT = s.sb("qT", [128, 8, TOK], BF16)
    kT = s.sb("kT", [128, 2, TH], BF16)
    vx = s.sb("vx", [128, NTH, 2, 65], BF16)
    bqkv = s.sb("bqkv_s", [128, 1280], F32)
    qkg = s.sb("qkg_s", [128, 2, 64], F32)
    esink = s.sb("esink", [128, 16], F32)
    invf = s.sb("invf_s", [128, 8], F32)
    posi = s.sb("posi", [128, NTH], I32)
    posf = s.sb("posf", [128, NTH], F32)
    masks = s.sb("masks_s", [128, 4, 128], BF16)
    s.dma("sp", bqkv, bqkv_d)
    s.dma("sp", qkg, qkg_d)
    s.dma("sp", esink, sink_d)
    s.dma("sp", invf, invf_d)
    s.dma("sp", posi, pos)
    s.dma("pool", masks, masks_d)
    b.cp(posf, posi)
    b.act(esink, esink, AF.Exp)
    b.memset(vx[:, :, :, 64:65], 1.0)
    s.mark()
    qf = s.sb("qf", [128, 1024], F32)
    kvf = s.sb("kvf", [128, 256], F32)
    sqq = s.sb("sqq", [128, 1024], F32)
    qn = s.sb("qn", [128, 1024], F32)
    qb = s.sb("qb", [128, 1024], BF16)
    kn = s.sb("kn", [128, 128], F32)
    kb2 = s.sb("kb2", [128, 2, 2, 64], BF16)
    rq = s.sb("rq", [128, 18], F32)
    ang = s.sb("ang", [128, 2, 8], F32)
    kq = s.sb("kq", [128, 2, 8], F32)
    ki = s.sb("ki", [128, 2, 8], I32)
    sc = s.sb("sc", [128, 2, 8], F32)
    rt = s.sb("rt", [128, 4, 16, 8], F32)
    def trans(j):
        own = j >= 1
        pb = PS[:, 3, :].bitcast(BF16).rearrange("p (a b) -> p a b", a=8)
        if own:
            for pr in range(8):
                b.tr(pb[:, pr, :], qb[:, pr * 128:(pr + 1) * 128], ident_b)
            b.cp(qT[:, :, (j - 1) * 128:j * 128], pb, eng="act")
        pk = PS[:, 4, :].bitcast(BF16).rearrange("p (a b) -> p a b", a=8)
        for g in range(2):
            b.tr(pk[:, g, :], kb2[:, g].rearrange("p a d -> p (a d)"), ident_b)
        b.cp(kT[:, :, j * 128:(j + 1) * 128], pk[:, 0:2, :])

    for j in range(NTH):
        own = j >= 1
        if own:
            for half in range(2):
                pq = PS[:, half, :]
                for c in range(DC):
                    b.mm(pq, xnT[:, c, j * 128:(j + 1) * 128], wq_s[:, c, half * 512:(half + 1) * 512], start=(c == 0), stop=(c == DC - 1))
        pkv = PS[:, 2, 0:256]
        for c in range(DC):
            b.mm(pkv, xnT[:, c, j * 128:(j + 1) * 128], wkv_s[:, c, :], start=(c == 0), stop=(c == DC - 1))
        if own:
            b.tt(qf, PS[:, 0:2, :].rearrange("p a b -> p (a b)"), bqkv[:, 0:1024], ALU.add)
        b.tt(kvf, pkv, bqkv[:, 1024:1280], ALU.add)
        if j > 0:
            trans(j - 1)
        b.ts(ang[:, 0, :], invf, posf[:, j:j + 1], None, ALU.mult)
        b.ts(ang[:, 1, :], ang[:, 0, :], 1.5707963267948966, None, ALU.add)
        b.ts(kq, ang, 1.0 / TWO_PI, None, ALU.mult)
        b.cp(ki, kq)
        b.cp(kq, ki)
        b.stt(ang, kq, -CW1, ang, ALU.mult, ALU.add)
        b.stt(ang, kq, -CW2, ang, ALU.mult, ALU.add)
        b.ts(kq, ang, 3.141592653589793, -TWO_PI, ALU.is_gt, ALU.mult)
        b.tt(ang, ang, kq, ALU.add)
        b.ts(kq, ang, -3.141592653589793, TWO_PI, ALU.is_lt, ALU.mult)
        b.tt(ang, ang, kq, ALU.add)
        b.act(sc, ang, AF.Sin)
        sinb = lambda nh: sc[:, 0, :].unsqueeze(1).broadcast_to([128, nh, 8])
        cosb = lambda nh: sc[:, 1, :].unsqueeze(1).broadcast_to([128, nh, 8])

        def norm_rot(src, nh, gidx, dst_f, dst_b_view, rcol0):
            sv = src.rearrange("p (h d) -> p h d", h=nh)
            b.tt(sqq[:, 0:nh * 64], src, src, ALU.mult)
            b.rsum(rq[:, rcol0:rcol0 + nh], sqq[:, 0:nh * 64].rearrange("p (h d) -> p h d", h=nh))
            b.ts(rq[:, rcol0:rcol0 + nh], rq[:, rcol0:rcol0 + nh], 1.0 / 64, EPS, ALU.mult, ALU.add)
            b.act(rq[:, rcol0:rcol0 + nh], rq[:, rcol0:rcol0 + nh], AF.Sqrt)
            b.recip(rq[:, rcol0:rcol0 + nh], rq[:, rcol0:rcol0 + nh])
            dv = dst_f.rearrange("p (h d) -> p h d", h=nh)
            b.tt(dv, sv, rq[:, rcol0:rcol0 + nh].unsqueeze(2).broadcast_to([128, nh, 64]), ALU.mult)
            b.tt(dv, dv, qkg[:, gidx, :].unsqueeze(1).broadcast_to([128, nh, 64]), ALU.mult)
            b.cp(dst_b_view, dv, eng="act")
            r1 = dv[:, :, 0:8]
            r2 = dv[:, :, 8:16]
            b.tt(rt[:, 0, 0:nh, :], r1, cosb(nh), ALU.mult)
            b.tt(rt[:, 1, 0:nh, :], r2, sinb(nh), ALU.mult)
            b.tt(rt[:, 2, 0:nh, :], r2, cosb(nh), ALU.mult)
            b.tt(rt[:, 3, 0:nh, :], r1, sinb(nh), ALU.mult)
            b.tt(dst_b_view[:, :, 0:8], rt[:, 0, 0:nh, :], rt[:, 1, 0:nh, :], ALU.subtract)
            b.tt(dst_b_view[:, :, 8:16], rt[:, 2, 0:nh, :], rt[:, 3, 0:nh, :], ALU.add)

        if own:
            norm_rot(qf, 16, 0, qn, qb.rearrange("p (h d) -> p h d", h=16), 0)
        norm_rot(kvf[:, 0:128], 2, 1, kn, kb2[:, :, 0, :], 16)
        b.cp(kb2[:, :, 1, :], kb2[:, :, 0, :])
        b.cp(vx[:, j, :, 0:64], kvf[:, 128:256].rearrange("p (g d) -> p g d", g=2), eng="act")
    trans(NTH - 1)
    s.release()
    b.dbg("qT", qT, [128, 8, TOK], BF16)
    b.dbg("kT", kT, [128, 2, TH], BF16)
    b.dbg("vx", vx, [128, NTH, 2, 65], BF16)
    if b.stop_after == "1d":
        return

    s.mark()
    PTs = [s.sb("PT%d" % i, [128, 2, 4, 128], BF16) for i in range(2)]
    attn = [s.sb("attn%d" % i, [128, 16, 64], BF16) for i in range(2)]
    den = s.sb("den", [128, 4, 4], F32)
    iters = [(blk, g, hh) for blk in range(NT) for g in range(2) for hh in range(2)]

    def scores(it):
        blk, g, hh = iters[it]
        st = PS[:, (it % 2) * 2:(it % 2) * 2 + 2, :]
        for kb in range(2):
            b.mm(st[:, kb, :], kT[hh * 64:(hh + 1) * 64, g, (blk + kb) * 128:(blk + kb + 1) * 128],
                 qT[hh * 64:(hh + 1) * 64, 4 * g:4 * g + 4, blk * 128:(blk + 1) * 128])

    scores(0)
    for it, (blk, g, hh) in enumerate(iters):
        at = attn[blk % 2]
        st = PS[:, (it % 2) * 2:(it % 2) * 2 + 2, :]
        PT = PTs[it % 2]
        b.act(PT.rearrange("p a h q -> p a (h q)"), st, AF.Exp, scale=0.125)
        if it + 1 < len(iters):
            scores(it + 1)
        mk = masks[:, 0:2, :] if blk == 0 else masks[:, 2:4, :]
        b.tt(PT, PT, mk.unsqueeze(2).broadcast_to([128, 2, 4, 128]), ALU.mult)
        po = PS[:, 4 + (it % 2), 0:260].rearrange("p (h d) -> p h d", h=4)
        for hd in range(4):
            for kb in range(2):
                b.mm(po[:, hd, :], PT[:, kb, hd, :], vx[:, blk + kb, g, :], start=(kb == 0), stop=(kb == 1))
        dn = den[:, it % 4, :]
        h0 = 8 * g + hh
        b.tt(dn, po[:, :, 64], esink[:, h0:h0 + 7:2], ALU.add)
        b.recip(dn, dn)
        b.tt(at[:, h0:h0 + 7:2, :], po[:, :, 0:64], dn.unsqueeze(2).broadcast_to([128, 4, 64]), ALU.mult)
        if g == 1 and hh == 1:
            pb = PS[:, 6 + (blk % 2), :].bitcast(BF16).rearrange("p (a b) -> p a b", a=8)
            for pr in range(8):
                b.tr(pb[:, pr, :], at[:, 2 * pr:2 * pr + 2, :].rearrange("p a d -> p (a d)"), ident_b)
            b.cp(mixedT[:, 8:16, blk * 128:(blk + 1) * 128], pb, eng="act")
    s.release()
    s.release()
    s.release()
    b.dbg("mixedT", mixedT, [128, DC, TOK], BF16)
    if b.stop_after == "1e":
        return
    _emit2(b, locals())


def _emit2(b, L):
    s = b.s
    PS = s.psum
    xin, xnT, mixedT = L["xin"], L["xnT"], L["mixedT"]
    ident_b, ident_f, iota128 = L["ident_b"], L["ident_f"], L["iota128"]
    wout_d, g2bc_d, x1_d, g_d, y_d = L["wout_d"], L["g2bc_d"], L["x1_d"], L["g_d"], L["y_d"]
    wq_d, keys_d, ut_d, v_d, iota_d = L["wq_d"], L["keys_d"], L["ut_d"], L["v_d"], L["iota_d"]
    xn2T = L["xn2T"]
    s.mark()
    wout = s.sb("wout_s", [128, 4, DC, 512], BF16)
    g2bc = s.sb("g2bc_s", [128, D], F32)
    ss2 = s.sb("ss2", [128, NT], F32)
    rs2 = s.sb("rs2", [128, NT], F32)
    for i in range(4):
        s.dma("pool", wout[:, i], wout_d[i])
    s.dma("sp", g2bc, g2bc_d)
    xts = [s.sb("xr%d" % i, [128, D], F32) for i in range(2)]
    x1s = [s.sb("x1t%d" % i, [128, D], F32) for i in range(2)]
    xnbs = [s.sb("xn2b%d" % i, [128, D], BF16) for i in range(2)]
    junk = s.sb("junk2", [128, D], BF16)
    def proj_part(j):
        xt = xts[j % 2]
        x1 = x1s[j % 2]
        s.dma("sp", xt, xin[(j + 1) * 128:(j + 2) * 128, :])
        for qd in range(4):
            pp = PS[:, 4 + qd, :]
            for c in range(DC):
                b.mm(pp, mixedT[:, c, j * 128:(j + 1) * 128], wout[:, qd, c, :], start=(c == 0), stop=(c == DC - 1))
            b.tt(x1[:, qd * 512:(qd + 1) * 512], pp, xt[:, qd * 512:(qd + 1) * 512], ALU.add)
        s.dma("sp", x1_d[j * 128:(j + 1) * 128, :], x1)
        b.norm_part(x1, g2bc, ss2[:, j:j + 1], rs2[:, j:j + 1], junk, xnbs[j % 2])

    proj_part(0)
    for j in range(NT):
        if j + 1 < NT:
            proj_part(j + 1)
        b.transpose_part(xnbs[j % 2], xn2T, j * 128, ident_b, (j % 2) * 2)
    s.release()
    s.release()
    b.dbg("xn2T", xn2T, [128, DC, TOK], BF16)
    if b.stop_after == "1f":
        if "x1" in b.debug:
            pass
        return
    _emit3(b, L, xn2T)


def _emit3(b, L, xn2T):
    s = b.s
    PS = s.psum
    ident_f, iota128 = L["ident_f"], L["iota128"]
    x1_d, g_d, y_d = L["x1_d"], L["g_d"], L["y_d"]
    wq_d, keys_d, ut_d, v_d = L["wq_d"], L["keys_d"], L["ut_d"], L["v_d"]
    NEG = -1.0e30
    s.mark()
    v16 = s.sb("v16", [128, NT, 16, 16], F32)
    i16 = s.sb("i16", [128, NT, 16, 16], U32)
    ET = s.sb("ET", [128, 3, TOK], F32)
    E1T = ET[:, 0, :]
    E2T = ET[:, 1, :]
    gT = ET[:, 2, :]
    s.mark()
    keysT = s.sb("keysT_s", [128, 16, 128], BF16)
    s.dma("pool", keysT, keys_d)
    wqs = [s.sb("wqs%d" % i, [128, DC, 128], BF16) for i in range(2)]
    qhs = [s.sb("qhs%d" % i, [128, TOK], BF16) for i in range(2)]
    tmr = [s.sb("tmr%d" % i, [128, 128], F32) for i in range(4)]
    k = 0
    for gi in range(16):
        w = wqs[gi % 2]
        s.dma("pool", w, wq_d[gi])
        qp = PS[:, (gi % 2) * 2:(gi % 2) * 2 + 2, :].rearrange("p a b -> p (a b)")
        for half in range(2):
            for c in range(DC):
                b.mm(qp[:, half * 512:(half + 1) * 512], w[:, c, :], xn2T[:, c, half * 512:(half + 1) * 512], start=(c == 0), stop=(c == DC - 1))
        qh = qhs[gi % 2]
        b.cp(qh, qp, eng="act")
        sp_ = PS[:, 4 + (gi % 2) * 2:6 + (gi % 2) * 2, :].rearrange("p a (t n) -> p (a t) n", n=128)
        for tl in range(NT):
            b.mm(sp_[:, tl, :], qh[:, tl * 128:(tl + 1) * 128], keysT[:, gi, :])
        for tp in range(NT // 4):
            chains = []
            for tl in range(4 * tp, 4 * tp + 4):
                sv = sp_[:, tl, :]
                tm = tmr[tl % 4]
                va = v16[:, tl, gi, 0:8]
                vb = v16[:, tl, gi, 8:16]
                ia = i16[:, tl, gi, 0:8]
                ib = i16[:, tl, gi, 8:16]
                chains.append([
                    (lambda e, va=va, sv=sv: e.max(out=va, in_=sv), [sv], [va]),
                    (lambda e, ia=ia, va=va, sv=sv: e.max_index(out=ia, in_max=va, in_values=sv), [va, sv], [ia]),
                    (lambda e, tm=tm, va=va, sv=sv: e.match_replace(out=tm, in_to_replace=va, in_values=sv, imm_value=NEG), [va, sv], [tm]),
                    (lambda e, vb=vb, tm=tm: e.max(out=vb, in_=tm), [tm], [vb]),
                    (lambda e, ib=ib, vb=vb, tm=tm: e.max_index(out=ib, in_max=vb, in_values=tm), [vb, tm], [ib]),
                ])
            for step in range(5):
                for ch in chains:
                    fn, rd, wr = ch[step]
                    s.op("dve", fn, reads=rd, writes=wr)
    s.release()
    b.dbg("v16", v16, [128, NT, 16, 16])
    b.dbg("i16", i16, [128, NT, 16, 16], U32)
    if b.stop_after == "2a":
        return

    s.mark()
    i16f = s.sb("i16f", [128, 16, 16], F32)
    cand = s.sb("cand", [128, 8, 256], F32)
    cd2 = [s.sb("cd2_%d" % i, [128, 256], F32) for i in range(4)]
    vals = s.sb("vals", [128, 8, 16], F32)
    cidx = s.sb("cidx", [128, 8, 16], U32)
    aku = s.sb("aku", [128, 128], U32)
    bku = s.sb("bku", [128, 128], U32)
    akf = s.sb("akf", [128, 128], F32)
    bkf = s.sb("bkf", [128, 128], F32)
    eq = s.sb("eq", [128, 128, 16], F32)
    E12 = s.sb("E12", [128, 3, 128], F32)
    dd = s.sb("dd", [128, 8, 16], F32)
    zz = s.sb("zz", [128, 8], F32)
    io16 = iota128[:, 0:16].unsqueeze(1).broadcast_to([128, 128, 16])
    NOH = 16
    oh1 = [s.sb("oh1_%d" % i, [128, 128], BF16) for i in range(NOH)]
    oh2 = [s.sb("oh2_%d" % i, [128, 128], BF16) for i in range(NOH)]
    gst = [s.sb("gst%d" % i, [128, 128, 128], BF16) for i in range(2)]
    iota_b = s.sb("iota_b", [128, 128], BF16)
    b.cp(iota_b, iota128)

    def gen_2b(tl):
        b.cp(i16f, i16[:, tl])
        yield
        vv = v16[:, tl].rearrange("p (h two) k -> p h two k", two=2)
        in0 = vv[:, :, 0, :].unsqueeze(3).broadcast_to([128, 8, 16, 16])
        in1 = vv[:, :, 1, :].unsqueeze(2).broadcast_to([128, 8, 16, 16])
        b.tt(cand.rearrange("p h (a c) -> p h a c", a=16), in0, in1, ALU.add)
        yield
        for hp in range(2):
            chains = []
            for hh in range(4 * hp, 4 * hp + 4):
                cv = cand[:, hh, :]
                c2 = cd2[hh % 4]
                va = vals[:, hh, 0:8]
                vb = vals[:, hh, 8:16]
                ia = cidx[:, hh, 0:8]
                ib = cidx[:, hh, 8:16]
                chains.append([
                    (lambda e, va=va, cv=cv: e.max(out=va, in_=cv), [cv], [va]),
                    (lambda e, ia=ia, va=va, cv=cv: e.max_index(out=ia, in_max=va, in_values=cv), [va, cv], [ia]),
                    (lambda e, c2=c2, va=va, cv=cv: e.match_replace(out=c2, in_to_replace=va, in_values=cv, imm_value=NEG), [va, cv], [c2]),
                    (lambda e, vb=vb, c2=c2: e.max(out=vb, in_=c2), [c2], [vb]),
                    (lambda e, ib=ib, vb=vb, c2=c2: e.max_index(out=ib, in_max=vb, in_values=c2), [vb, c2], [ib]),
                ])
            for step in range(5):
                for ch in chains:
                    fn, rd, wr = ch[step]
                    s.op("dve", fn, reads=rd, writes=wr)
                    yield
        cf = cidx.rearrange("p h k -> p (h k)")
        s.op("dve", lambda e, cf=cf: e.tensor_single_scalar(out=aku, in_=cf, scalar=4, op=ALU.logical_shift_right), reads=[cf], writes=[aku])
        yield
        s.op("dve", lambda e, cf=cf: e.tensor_single_scalar(out=bku, in_=cf, scalar=15, op=ALU.bitwise_and), reads=[cf], writes=[bku])
        yield
        b.cp(akf, aku)
        yield
        b.cp(bkf, bku)
        yield
        i16v = i16f.rearrange("p (h two) k -> p h two k", two=2)
        for which, (kf, par) in enumerate(((akf, 0), (bkf, 1))):
            b.tt(eq, kf.unsqueeze(2).broadcast_to([128, 128, 16]), io16, ALU.is_equal)
            yield
            e4 = eq.rearrange("p (h k) a -> p h k a", h=8)
            b.tt(e4, e4, i16v[:, :, par, :].unsqueeze(2).broadcast_to([128, 8, 16, 16]), ALU.mult)
            yield
            b.rsum(E12[:, which, :], eq)
            yield
        b.tt(dd, vals, vals[:, :, 0:1].broadcast_to([128, 8, 16]), ALU.subtract)
        yield
        b.act(dd, dd, AF.Exp)
        b.rsum(zz, dd)
        yield
        b.recip(zz, zz)
        yield
        b.tt(E12[:, 2, :].rearrange("p (h k) -> p h k", h=8), dd, zz.unsqueeze(2).broadcast_to([128, 8, 16]), ALU.mult)
        yield
        pt = PS[:, tl % 2, 0:384].rearrange("p (a b) -> p a b", a=3)
        for i in range(3):
            b.tr(pt[:, i, :], E12[:, i, :], ident_f)
        b.cp(ET[:, :, tl * 128:(tl + 1) * 128], pt, eng="act")
        yield

    for _ in gen_2b(0):
        pass
    for tl in range(NT):
        G = gst[tl % 2]
        nxt = gen_2b(tl + 1) if tl + 1 < NT else None
        for t8 in range(16):
            gp = PS[:, 4 + (t8 % 2) * 2:6 + (t8 % 2) * 2, :].rearrange("p a (t e) -> p (a t) e", e=128)
            for ti in range(8):
                tok = t8 * 8 + ti
                t = tl * 128 + tok
                o1 = oh1[tok % NOH]
                o2 = oh2[tok % NOH]
                b.ts(o1, iota_b, E1T[:, t:t + 1], None, ALU.is_equal)
                b.ts(o2, iota_b, E2T[:, t:t + 1], gT[:, t:t + 1], ALU.is_equal, ALU.mult)
                b.mm(gp[:, ti, :], o2, o1)
                if nxt is not None and tok % 2 == 1:
                    next(nxt, None)
            b.cp(G[:, :, t8 * 8:(t8 + 1) * 8], gp.rearrange("p t e -> p e t"), eng="act")
        if nxt is not None:
            for _ in nxt:
                pass
        for q4 in range(4):
            s.dma("sp", g_d[q4 * 32:(q4 + 1) * 32, :, tl * 128:(tl + 1) * 128].rearrange("c e t -> e c t"), G[:, q4 * 32:(q4 + 1) * 32, :])
    s.release()
    b.dbg("E1T", E1T, [128, TOK])
    b.dbg("E2T", E2T, [128, TOK])
    b.dbg("gT", gT, [128, TOK])
    s.release()
    if b.stop_after == "2c":
        return

    yacc = s.sb("yacc", [128, NT, D], F32)
    NCH = 128
    CG = 4
    NG = NCH // CG
    uts = [s.sb("ut%d" % i, [128, DC, 128], BF16) for i in range(2 * CG)]
    vgs = [s.sb("vg%d" % i, [128, CG, D], BF16) for i in range(2)]
    gts = [s.sb("gt%d" % i, [128, TOK], BF16) for i in range(2 * CG)]
    gls = [s.sb("gl%d" % i, [128, TOK], F32) for i in range(2)]
    aTs = [s.sb("aT%d" % i, [128, CG, TOK], BF16) for i in range(2)]

    def load_v(g):
        for ci in range(CG):
            c = g * CG + ci
            s.dma("pool", vgs[g % 2][:, ci, :], v_d[c * 128:(c + 1) * 128, :])

    def load_u(g):
        for ci in range(CG):
            c = g * CG + ci
            s.dma("pool", uts[c % (2 * CG)], ut_d[c])
            s.dma("sp", gts[c % (2 * CG)], g_d[c])

    def h_group(g):
        for ci in range(CG):
            c = g * CG + ci
            u = uts[c % (2 * CG)]
            hp = PS[:, (c % 2) * 2:(c % 2) * 2 + 2, :].rearrange("p a b -> p (a b)")
            for half in range(2):
                for dc in range(DC):
                    b.mm(hp[:, half * 512:(half + 1) * 512], u[:, dc, :], xn2T[:, dc, half * 512:(half + 1) * 512], start=(dc == 0), stop=(dc == DC - 1))
            gl = gls[c % 2]
            b.act(gl, hp, AF.Gelu)
            b.tt(aTs[g % 2][:, ci, :], gl, gts[c % (2 * CG)], ALU.mult)

    ycnt = [0]

    def y_group(g):
        a = aTs[g % 2]
        vg = vgs[g % 2]
        for tl in range(NT):
            for qd in range(4):
                yp = PS[:, 4 + (ycnt[0] % 4), :]
                ycnt[0] += 1
                for ci in range(CG):
                    b.mm(yp, a[:, ci, tl * 128:(tl + 1) * 128], vg[:, ci, qd * 512:(qd + 1) * 512], start=(ci == 0), stop=(ci == CG - 1))
                ya = yacc[:, tl, qd * 512:(qd + 1) * 512]
                b.tt(ya, ya, yp, ALU.add)

    ng = NG if b.stop_after != "3s" else 2
    load_u(0)
    if ng > 1:
        load_u(1)
    load_v(0)
    for j in range(NT):
        s.dma("sp", yacc[:, j, :], x1_d[j * 128:(j + 1) * 128, :])
    if ng > 1:
        load_v(1)
    for g in range(ng):
        h_group(g)
        if g >= 1:
            y_group(g - 1)
        if g + 1 < ng and g >= 1:
            load_v(g + 1)
        if g + 2 < ng:
            load_u(g + 2)
    y_group(ng - 1)
    for j in range(NT):
        s.dma("sp", y_d[j * 128:(j + 1) * 128, :], yacc[:, j, :], is_output=True)


def _bc(v, n=128):
    return np.ascontiguousarray(np.broadcast_to(np.asarray(v, np.float32).reshape(1, -1), (n, v.size)))


def prepare_inputs(x, positions, norm1_g, w_in, b_in, conv_dw_w, conv_dw_b, conv_ln_g, conv_ln_b,
                   q_norm_g, k_norm_g, attn_sinks, w_out, norm2_g, peer_w_q, peer_sub_keys,
                   peer_u, peer_v):
    f32 = np.float32
    x = np.asarray(x, f32).reshape(SEQ, D)
    positions = np.asarray(positions).reshape(SEQ).astype(np.int32)
    w_in = np.asarray(w_in, f32)
    b_in = np.asarray(b_in, f32)
    shared = {}
    shared["g1bc"] = _bc(norm1_g)
    shared["g2bc"] = _bc(norm2_g)
    w_l = w_in.reshape(DC, 128, 3328).transpose(1, 0, 2)
    wconv = np.empty((8, 128, DC, 256), f32)
    for cp in range(8):
        wconv[cp, :, :, 0:128] = w_l[:, :, cp * 128:(cp + 1) * 128]
        wconv[cp, :, :, 128:256] = w_l[:, :, 1024 + cp * 128:1024 + (cp + 1) * 128]
    shared["wconv"] = wconv
    shared["wqkv"] = np.ascontiguousarray(w_l[:, :, 2048:3328])
    shared["bconv"] = np.ascontiguousarray(b_in[0:2048].reshape(16, 128).T)
    shared["bqkvbc"] = _bc(b_in[2048:3328])
    shared["convw"] = np.ascontiguousarray(np.asarray(conv_dw_w, f32).reshape(CONV_K, 8, 128).transpose(2, 1, 0))
    cv = np.stack([np.asarray(a, f32).reshape(8, 128).T for a in (conv_dw_b, conv_ln_g, conv_ln_b)], axis=1)
    shared["convvec"] = np.ascontiguousarray(cv)
    shared["qkgbc"] = np.ascontiguousarray(np.stack([_bc(q_norm_g), _bc(k_norm_g)], axis=1))
    shared["sinkbc"] = _bc(attn_sinks)
    inv_freq = (500000.0 ** (-np.arange(0, 16, 2, dtype=np.float32) / 16)).astype(f32)
    shared["invfbc"] = _bc(inv_freq)
    shared["wout"] = np.ascontiguousarray(np.asarray(w_out, f32).reshape(DC, 128, 4, 512).transpose(2, 1, 0, 3))
    wq_l = np.asarray(peer_w_q, f32).reshape(DC, 128, 16, 128).transpose(2, 1, 0, 3)
    shared["wq"] = np.ascontiguousarray(wq_l)
    keys = np.asarray(peer_sub_keys, f32).reshape(16, 128, 128)
    shared["keysT"] = np.ascontiguousarray(keys.transpose(2, 0, 1))
    u = np.asarray(peer_u, f32).reshape(128, 128, DC, 128)
    shared["ut"] = np.ascontiguousarray(u.transpose(0, 3, 2, 1))
    shared["pv"] = np.ascontiguousarray(np.asarray(peer_v, f32))
    io = np.empty((128, 128 + 2048), f32)
    io[:, 0:128] = np.arange(128, dtype=f32)[None, :]
    io[:, 128:] = (np.arange(2048) % 16).astype(f32)[None, :]
    shared["iotas"] = io
    si = np.arange(128)[:, None]
    qi = np.arange(128)[None, :]
    mprev = (si > qi).astype(f32)
    mcur = (si <= qi).astype(f32)
    in_maps = []
    for c in range(NCORES):
        m = dict(shared)
        t0 = c * TOK
        xin = np.zeros((TH, D), f32)
        xin[128:] = x[t0:t0 + TOK]
        pp = np.zeros((TH,), np.int32)
        pp[128:] = positions[t0:t0 + TOK]
        if c > 0:
            xin[:128] = x[t0 - 128:t0]
            pp[:128] = positions[t0 - 128:t0]
        m["xin"] = xin
        m["pos"] = np.ascontiguousarray(pp.reshape(NTH, 128).T)
        first = mprev if c > 0 else np.zeros_like(mprev)
        m["masks"] = np.ascontiguousarray(np.stack([first, mcur, mprev, mcur], axis=1))
        m["halomask"] = np.full((128, 128), 1.0 if c > 0 else 0.0, f32)
        in_maps.append(m)
    return in_maps


_PROGRAM = None


def kernel(**inputs):
    global _PROGRAM
    in_maps = prepare_inputs(**inputs)
    if _PROGRAM is None:
        _PROGRAM = build_program()[0]
    res = run_bass_kernel_spmd(_PROGRAM, in_maps, core_ids=list(range(NCORES)))
    out = np.concatenate([r["y"] for r in res.results], axis=0)
    return out.reshape(1, SEQ, D).astype(np.float32)
```
